# Optimizing a Trainium2 kernel written in Bass

```python
import math
import jax, jax.numpy as jnp
from jax import lax
import numpy as np

D_MODEL = 1024
BATCH = 8
SEQ = 2048
DEPTH = 2

MEM_LEN = 256
BLOCK = 128
HEAD_DIM = 64
EPS = 1e-6
A_HEADS = 8
A_WIDTH = A_HEADS * HEAD_DIM
B_GROUPS = 4
B_GROUP_DIM = 128
B_WIDTH = B_GROUPS * B_GROUP_DIM
CHUNK = 128
EVEN_IN = 3 * A_WIDTH + A_HEADS + 2 * B_WIDTH
EVEN_SPLITS = [A_WIDTH, 2 * A_WIDTH, 3 * A_WIDTH, 3 * A_WIDTH + A_HEADS, 3 * A_WIDTH + A_HEADS + B_WIDTH]
C_WIDTH = 512
CONV_W = 3
D_HEADS = 4
D_WIDTH = D_HEADS * 2 * HEAD_DIM
ODD_IN = 3 * C_WIDTH + 3 * D_WIDTH
ODD_SPLITS = [C_WIDTH, 2 * C_WIDTH, 3 * C_WIDTH, 3 * C_WIDTH + D_WIDTH, 3 * C_WIDTH + 2 * D_WIDTH]
N_BUCKETS = 32
MAX_DIST = 128
X_HEADS = 4
X_HEAD_DIM = 128
X_WIDTH = X_HEADS * X_HEAD_DIM
FF_DENSE = 2816
N_EXPERTS = 8
TOP_K = 2
FF_EXPERT = 3584
N_EVEN = (DEPTH + 1) // 2
N_ODD = DEPTH // 2

kernel_name = "hybrid_fox_gmlp_shortconv_diffattn_moe"


def rmsnorm(x, g):
    xf = x.astype(jnp.float32)
    y = xf * lax.rsqrt(jnp.mean(xf * xf, axis=-1, keepdims=True) + EPS)
    return (y * g.astype(jnp.float32)).astype(x.dtype)


def t5_bucket(dist):
    n = jnp.maximum(dist, 0)
    max_exact = N_BUCKETS // 2
    nf = jnp.maximum(n, 1).astype(jnp.float32)
    large = max_exact + (jnp.log(nf / max_exact) / math.log(MAX_DIST / max_exact)
                         * (N_BUCKETS - max_exact)).astype(jnp.int32)
    large = jnp.minimum(large, N_BUCKETS - 1)
    return jnp.where(n < max_exact, n, large)


def fox_attention(q, k, v, log_f):
    seq = q.shape[2]
    c = jnp.cumsum(log_f, axis=-1)
    scale = HEAD_DIM ** -0.5
    outs = []
    for blk in range(seq // BLOCK):
        qs, qe = blk * BLOCK, (blk + 1) * BLOCK
        logits = jnp.einsum("bhqd,bhkd->bhqk", q[:, :, qs:qe], k[:, :, :qe]).astype(jnp.float32) * scale
        logits = logits + (c[:, :, qs:qe, None] - c[:, :, None, :qe])
        causal = (qs + jnp.arange(BLOCK))[:, None] >= jnp.arange(qe)[None, :]
        logits = jnp.where(causal, logits, -jnp.inf)
        p = jax.nn.softmax(logits, axis=-1).astype(v.dtype)
        outs.append(jnp.einsum("bhqk,bhkd->bhqd", p, v[:, :, :qe]))
    return jnp.concatenate(outs, axis=2)


def spatial_gating(u, v, sgu_norm, w_s, b_s):
    bsz, seq, _ = u.shape
    u = jax.nn.gelu(u).reshape(bsz, seq, B_GROUPS, B_GROUP_DIM)
    v = rmsnorm(jax.nn.gelu(v).reshape(bsz, seq, B_GROUPS, B_GROUP_DIM), sgu_norm)
    v = v.reshape(bsz, seq // CHUNK, CHUNK, B_GROUPS, B_GROUP_DIM)
    tri = jnp.tril(jnp.ones((CHUNK, CHUNK), dtype=bool))
    w = jnp.where(tri[None], w_s, jnp.zeros_like(w_s))
    mixed = jnp.einsum("gts,bcsgd->bctgd", w, v) + b_s.T[None, None, :, :, None]
    return (u * mixed.reshape(bsz, seq, B_GROUPS, B_GROUP_DIM)).reshape(bsz, seq, B_WIDTH)


def short_conv(b_gate, c_gate, x_in, conv_w):
    xc = c_gate * x_in
    y = lax.conv_general_dilated(xc, conv_w[:, None, :], window_strides=(1,),
                                 padding=[(CONV_W - 1, 0)],
                                 dimension_numbers=("NWC", "WIO", "NWC"),
                                 feature_group_count=C_WIDTH)
    return b_gate * y


def diff_attention(q, k, v, rel_bias, lam, lam_init, subln):
    seq = q.shape[3]
    scale = HEAD_DIM ** -0.5
    outs = []
    for blk in range(seq // BLOCK):
        qs, qe = blk * BLOCK, (blk + 1) * BLOCK
        dist = (qs + jnp.arange(BLOCK))[:, None] - jnp.arange(qe)[None, :]
        bias = rel_bias[t5_bucket(dist)].transpose(2, 0, 1).astype(jnp.float32)
        logits = jnp.einsum("bhiqd,bhikd->bhiqk", q[:, :, :, qs:qe], k[:, :, :, :qe]).astype(jnp.float32) * scale
        logits = jnp.where(dist >= 0, logits + bias[None, :, None], -jnp.inf)
        p = jax.nn.softmax(logits, axis=-1)
        p = p[:, :, 0] - lam * p[:, :, 1]
        outs.append(jnp.einsum("bhqk,bhkd->bhqd", p.astype(v.dtype), v[:, :, :qe]))
    o = jnp.concatenate(outs, axis=2)
    return rmsnorm(o, subln) * (1.0 - lam_init)


def even_mixer(h, w_in, b_f, sgu_norm, w_s, b_s, w_out):
    bsz, seq, _ = h.shape
    qa, ka, va, fa, ub, vb = jnp.split(h @ w_in, EVEN_SPLITS, axis=-1)
    heads = lambda t: t.reshape(bsz, seq, A_HEADS, HEAD_DIM).transpose(0, 2, 1, 3)
    log_f = jax.nn.log_sigmoid(fa.astype(jnp.float32) + b_f.astype(jnp.float32)).transpose(0, 2, 1)
    a = fox_attention(heads(qa), heads(ka), heads(va), log_f)
    a = a.transpose(0, 2, 1, 3).reshape(bsz, seq, A_WIDTH)
    g = spatial_gating(ub, vb, sgu_norm, w_s, b_s)
    return jnp.concatenate([a, g], axis=-1) @ w_out


def odd_mixer(h, w_in, conv_w, lam_q1, lam_k1, lam_q2, lam_k2, subln, w_out, rel_bias, lam_init):
    bsz, seq, _ = h.shape
    bg, cg, xi, qd, kd, vd = jnp.split(h @ w_in, ODD_SPLITS, axis=-1)
    c_out = short_conv(bg, cg, xi, conv_w)
    q = qd.reshape(bsz, seq, D_HEADS, 2, HEAD_DIM).transpose(0, 2, 3, 1, 4)
    k = kd.reshape(bsz, seq, D_HEADS, 2, HEAD_DIM).transpose(0, 2, 3, 1, 4)
    v = vd.reshape(bsz, seq, D_HEADS, 2 * HEAD_DIM).transpose(0, 2, 1, 3)
    lam = (jnp.exp(jnp.sum(lam_q1.astype(jnp.float32) * lam_k1.astype(jnp.float32)))
           - jnp.exp(jnp.sum(lam_q2.astype(jnp.float32) * lam_k2.astype(jnp.float32))) + lam_init)
    d_out = diff_attention(q, k, v, rel_bias, lam, lam_init, subln)
    d_out = d_out.transpose(0, 2, 1, 3).reshape(bsz, seq, D_WIDTH)
    return jnp.concatenate([c_out, d_out], axis=-1) @ w_out


def memory_cross_attention(h, mem_n, wq, wkv, wo):
    bsz, seq, _ = h.shape
    m = mem_n.shape[1]
    q = (h @ wq).reshape(bsz, seq, X_HEADS, X_HEAD_DIM)
    k, v = jnp.split(mem_n @ wkv, 2, axis=-1)
    k = k.reshape(bsz, m, X_HEADS, X_HEAD_DIM)
    v = v.reshape(bsz, m, X_HEADS, X_HEAD_DIM)
    logits = jnp.einsum("bshd,bmhd->bhsm", q, k).astype(jnp.float32) * (X_HEAD_DIM ** -0.5)
    p = jax.nn.softmax(logits, axis=-1).astype(v.dtype)
    o = jnp.einsum("bhsm,bmhd->bshd", p, v).reshape(bsz, seq, X_WIDTH)
    return o @ wo


def swiglu(h, w13, w2):
    gate, up = jnp.split(h @ w13, 2, axis=-1)
    return (jax.nn.silu(gate) * up) @ w2


def moe_swiglu(h, w_router, w13, w2):
    logits = (h @ w_router).astype(jnp.float32)
    top_v, top_i = lax.top_k(logits, TOP_K)
    wts = jax.nn.softmax(top_v, axis=-1)
    gate = jnp.sum(jax.nn.one_hot(top_i, N_EXPERTS, dtype=jnp.float32) * wts[..., None], axis=-2)
    y = jnp.zeros_like(h)
    for e in range(N_EXPERTS):
        y = y + gate[..., e:e + 1].astype(h.dtype) * swiglu(h, w13[e], w2[e])
    return y


def setup_inputs(seed: int = 0) -> dict:
    key = jax.random.key(seed)
    ks = iter(jax.random.split(key, 64))

    def nrm(shape, scale):
        return scale * jax.random.normal(next(ks), shape, jnp.float32)

    def gain(shape):
        return 1.0 + 0.05 * jax.random.normal(next(ks), shape, jnp.float32)

    d = D_MODEL
    return {
        "x": nrm((BATCH, SEQ, d), 1.0),
        "mem": nrm((BATCH, MEM_LEN, d), 1.0),
        "rel_bias": nrm((N_BUCKETS, D_HEADS), 0.5),
        "mem_norm": gain((d,)),
        "final_norm": gain((d,)),
        "ev_norm": gain((N_EVEN, d)),
        "ev_w_in": nrm((N_EVEN, d, EVEN_IN), d ** -0.5),
        "ev_b_f": 2.0 + nrm((N_EVEN, A_HEADS), 0.1),
        "ev_sgu_norm": gain((N_EVEN, B_GROUPS, B_GROUP_DIM)),
        "ev_w_s": nrm((N_EVEN, B_GROUPS, CHUNK, CHUNK), CHUNK ** -0.5),
        "ev_b_s": 1.0 + nrm((N_EVEN, B_GROUPS, CHUNK), 0.1),
        "ev_w_out": nrm((N_EVEN, A_WIDTH + B_WIDTH, d), (A_WIDTH + B_WIDTH) ** -0.5),
        "ffn_w13": nrm((N_EVEN, d, 2 * FF_DENSE), d ** -0.5),
        "ffn_w2": nrm((N_EVEN, FF_DENSE, d), FF_DENSE ** -0.5),
        "od_norm": gain((N_ODD, d)),
        "od_w_in": nrm((N_ODD, d, ODD_IN), d ** -0.5),
        "od_conv_w": nrm((N_ODD, CONV_W, C_WIDTH), CONV_W ** -0.5),
        "od_lam_q1": nrm((N_ODD, HEAD_DIM), 0.1),
        "od_lam_k1": nrm((N_ODD, HEAD_DIM), 0.1),
        "od_lam_q2": nrm((N_ODD, HEAD_DIM), 0.1),
        "od_lam_k2": nrm((N_ODD, HEAD_DIM), 0.1),
        "od_subln": gain((N_ODD, 2 * HEAD_DIM)),
        "od_w_out": nrm((N_ODD, C_WIDTH + D_WIDTH, d), (C_WIDTH + D_WIDTH) ** -0.5),
        "moe_router": nrm((N_ODD, d, N_EXPERTS), d ** -0.5),
        "moe_w13": nrm((N_ODD, N_EXPERTS, d, 2 * FF_EXPERT), d ** -0.5),
        "moe_w2": nrm((N_ODD, N_EXPERTS, FF_EXPERT, d), FF_EXPERT ** -0.5),
        "x_norm": gain((DEPTH, d)),
        "x_wq": nrm((DEPTH, d, X_WIDTH), d ** -0.5),
        "x_wkv": nrm((DEPTH, d, 2 * X_WIDTH), d ** -0.5),
        "x_wo": nrm((DEPTH, X_WIDTH, d), X_WIDTH ** -0.5),
        "ffn_norm": gain((DEPTH, d)),
    }


def reference(x, mem, rel_bias, mem_norm, final_norm,
              ev_norm, ev_w_in, ev_b_f, ev_sgu_norm, ev_w_s, ev_b_s, ev_w_out,
              ffn_w13, ffn_w2,
              od_norm, od_w_in, od_conv_w, od_lam_q1, od_lam_k1, od_lam_q2, od_lam_k2, od_subln, od_w_out,
              moe_router, moe_w13, moe_w2,
              x_norm, x_wq, x_wkv, x_wo, ffn_norm):
    mem_n = rmsnorm(mem, mem_norm)
    for layer in range(DEPTH):
        j = layer // 2
        if layer % 2 == 0:
            h = rmsnorm(x, ev_norm[j])
            x = x + even_mixer(h, ev_w_in[j], ev_b_f[j], ev_sgu_norm[j], ev_w_s[j], ev_b_s[j], ev_w_out[j])
        else:
            lam_init = 0.8 - 0.6 * math.exp(-0.3 * layer)
            h = rmsnorm(x, od_norm[j])
            x = x + odd_mixer(h, od_w_in[j], od_conv_w[j], od_lam_q1[j], od_lam_k1[j], od_lam_q2[j],
                              od_lam_k2[j], od_subln[j], od_w_out[j], rel_bias, lam_init)
        h = rmsnorm(x, x_norm[layer])
        x = x + memory_cross_attention(h, mem_n, x_wq[layer], x_wkv[layer], x_wo[layer])
        h = rmsnorm(x, ffn_norm[layer])
        if layer % 2 == 0:
            x = x + swiglu(h, ffn_w13[j], ffn_w2[j])
        else:
            x = x + moe_swiglu(h, moe_router[j], moe_w13[j], moe_w2[j])
    return rmsnorm(x, final_norm)
```

```python
import math
from contextlib import ExitStack
import numpy as np
import concourse.bass as bass
import concourse.mybir as mybir
from concourse.bass_utils import run_bass_kernel_spmd

F32 = mybir.dt.float32
BF16 = mybir.dt.bfloat16
AF = mybir.ActivationFunctionType
ALU = mybir.AluOpType
AX = mybir.AxisListType

T = 2048
NR = 4
D = 1024
KC = 8
EPS = 1e-6
FF_DENSE = 2816
FF_EXP = 3584
NEXP = 8
LAM_INIT = 0.8 - 0.6 * math.exp(-0.3 * 1)
RING_SLOTS = 4
RING_ELEMS = 4096
DLY_A = 2
DLY_B = 1
G_EV, G_X0, G_F0, G_OD, G_X1, G_F1, G_FIN = range(7)


class Eng:
    def __init__(self, raw, sem, name):
        self.raw, self.sem, self.name = raw, sem, name
        self.n = 0
        self.waited = {}
        self.last = None
        self.last_marked = True

    def wait(self, *evs):
        for ev in evs:
            if ev is None:
                continue
            if isinstance(ev, (list, tuple)) and ev and isinstance(ev[0], (list, tuple)):
                self.wait(*ev)
                continue
            sem, val = ev
            key = id(sem)
            if self.waited.get(key, 0) >= val:
                continue
            self.raw.wait_ge(sem, val)
            self.waited[key] = val

    def mark(self, inst):
        self.n += 1
        inst.then_inc(self.sem, 1)
        self.last = inst
        self.last_marked = True
        ev = (self.sem, self.n)
        return ev

    def nomark(self, inst):
        self.last = inst
        self.last_marked = False

    def now(self):
        if not self.last_marked and self.last is not None:
            return self.mark(self.last)
        if self.n == 0:
            return None
        return (self.sem, self.n)


class Buf:
    def __init__(self):
        self.w = None
        self.reads = []

    def rd(self, eng):
        eng.wait(self.w)

    def rd_done(self, ev):
        self.reads.append(ev)

    def wr(self, eng):
        eng.wait(self.w, *self.reads)

    def wr_done(self, ev):
        self.w = ev
        self.reads = []


class K:
    pass


def build_nc(stop_after=None, dbg=False):
    nc = bass.Bass("TRN2", target_bir_lowering=False)
    k = K()
    k.nc = nc
    es = ExitStack()
    k.es = es

    def din(name, shape):
        return nc.dram_tensor(name, list(shape), F32, kind="ExternalInput").ap()

    x_d = din("x", [T, D])
    mem_d = din("mem", [256, D])
    ident_d = din("ident", [128, 128])
    tri_d = din("tri", [128, 128])
    gv_d = din("gv", [128, 7 * 8])
    memg_d = din("memg", [128, D])
    bf8_d = din("bf8", [8, 1])
    sgu_d = din("sgu", [128, 512])
    wsT_d = din("wsT", [128, 512])
    bs_d = din("bs", [1, 512])
    convw_d = din("convw", [128, 12])
    lamv_d = din("lamv", [128, 256])
    subln_d = din("subln", [128, 1])
    btab_d = din("btab", [128, 4 * 2 * 128])
    rel31_d = din("rel31", [128, 4])
    wr_d = din("wr", [128, 64])
    sel_d = din("sel", [8, 8 * 128])
    sel72_d = din("sel72", [128, 8 * 128])
    ev_w_in = din("ev_w_in", [D, 2568])
    ev_w_out = din("ev_w_out", [D, D])
    ffn_w13 = din("ffn_w13", [D, 2 * FF_DENSE])
    ffn_w2 = din("ffn_w2", [FF_DENSE, D])
    od_w_in = din("od_w_in", [D, 3072])
    od_w_out = din("od_w_out", [D, D])
    moe_w13 = din("moe_w13", [NEXP * D, 2 * FF_EXP])
    moe_w2 = din("moe_w2", [NEXP * FF_EXP, D])
    x_wq = din("x_wq", [2 * D, 512])
    x_wkv = din("x_wkv", [2 * D, D])
    x_wo = din("x_wo", [2 * 512, D])
    out_d = nc.dram_tensor("out", [T, D], F32, kind="ExternalOutput").ap()

    uniq = [0]

    def sb(name, shape, dt=F32, stack=None):
        uniq[0] += 1
        return (stack or es).enter_context(nc.sbuf_tensor(f"sb{uniq[0]}_{name}", list(shape), dt))

    def sem(name):
        uniq[0] += 1
        return es.enter_context(nc.semaphore(f"{name}_{uniq[0]}"))

    PE = Eng(nc.tensor, sem("s_pe"), "pe")
    ACT = Eng(nc.scalar, sem("s_act"), "act")
    DVE = Eng(nc.vector, sem("s_dve"), "dve")
    POOL = Eng(nc.gpsimd, sem("s_pool"), "pool")
    SP = Eng(nc.sync, sem("s_sp"), "sp")
    engs = [PE, ACT, DVE, POOL, SP]
    dma_pending = []

    def barrier():
        evs = [e.now() for e in engs] + list(dma_pending)
        for e in engs:
            e.wait(*evs)
        del dma_pending[:]

    xT = sb("xT", [128, KC, T])
    memT = sb("memT", [128, KC, 256], BF16)
    ident = sb("ident_s", [128, 128])
    tri = sb("tri_s", [128, 128])
    tri_bf = sb("tri_bf", [128, 128], BF16)
    ones_bf = sb("ones_bf", [128, 128], BF16)
    gv = sb("gv_s", [128, 7, 8])
    rstd_tok = sb("rstd_tok", [128, 16])
    ring_t = [sb(f"ring{i}", [128, RING_ELEMS], BF16) for i in range(RING_SLOTS)]
    ring_sem = [sem(f"s_ring{i}") for i in range(RING_SLOTS)]
    ps_banks = [es.enter_context(nc.psum_tensor(f"ps{i}", [128, 512], F32)) for i in range(8)]
    s_setup = sem("s_setup")
    s_misc = sem("s_misc")
    setup_n = [0]

    def setup_dma(dst, src, eng=None):
        (eng or nc.sync).dma_start(out=dst, in_=src).then_inc(s_setup, 16)
        setup_n[0] += 1

    class Bank:
        def __init__(self, t, i):
            self.t, self.i = t, i
            self.buf = Buf()
            self.held = False
            self.stamp = 0
    banks = [Bank(t, i) for i, t in enumerate(ps_banks)]
    bank_clock = [0]

    def ps_alloc():
        free = [b for b in banks if not b.held]
        assert free, "no free PSUM bank"
        b = min(free, key=lambda b: b.stamp)
        b.held = True
        return b

    def ps_free(b, *evs):
        for ev in evs:
            b.buf.rd_done(ev)
        b.held = False
        bank_clock[0] += 1
        b.stamp = bank_clock[0]

    class Ring:
        def __init__(self):
            self.plan = []
            self.load_ev = {}
            self.released = {}
            self.issued = 0
            self.pos = 0
            self.loads = [0] * RING_SLOTS

        def add(self, W, r0, kc, c0, n):
            return self.add_multi([(W, r0, kc, c0, n)])

        def add_multi(self, parts):
            ent = []
            off = 0
            for (W, r0, kc, c0, n) in parts:
                src = W[r0:r0 + kc * 128, c0:c0 + n].rearrange("(k p) n -> p k n", p=128)
                ent.append((src, kc, n, off))
                off += kc * n
            assert off <= RING_ELEMS, off
            self.plan.append(ent)
            return len(self.plan) - 1

        def _try_issue(self):
            while self.issued < len(self.plan) and self.issued < self.pos + RING_SLOTS:
                j = self.issued
                s = j % RING_SLOTS
                if j >= RING_SLOTS:
                    if (j - RING_SLOTS) not in self.released:
                        break
                    POOL.wait(self.released[j - RING_SLOTS])
                for (src, kc, n, off) in self.plan[j]:
                    dst = ring_t[s][:, off:off + kc * n].rearrange("p (k n) -> p k n", k=kc)
                    nc.gpsimd.dma_start(out=dst, in_=src).then_inc(ring_sem[s], 16)
                    self.loads[s] += 1
                self.load_ev[j] = (ring_sem[s], 16 * self.loads[s])
                self.issued += 1

        def get(self, j):
            assert j == self.pos, (j, self.pos)
            self._try_issue()
            assert j in self.load_ev
            PE.wait(self.load_ev[j])
            self.pos += 1
            views = [ring_t[j % RING_SLOTS][:, off:off + kc * n].rearrange("p (k n) -> p k n", k=kc)
                     for (src, kc, n, off) in self.plan[j]]
            return views[0] if len(views) == 1 else views

        def release(self, j, ev):
            self.released[j] = ev
            self._try_issue()

    ring = Ring()

    P = {}

    def plan_cols(name, W, r0, kc, cols):
        P[name] = [(ring.add(W, r0, kc, c0, n), n) for (c0, n) in cols]

    def chunk_groups(c_start, nchunks, per=4):
        out = []
        j = 0
        while j < nchunks:
            n = min(per, nchunks - j)
            out.append((c_start + j * 128, n * 128))
            j += n
        return out

    plan_cols("l0_q", ev_w_in, 0, 8, [(0, 512)])
    plan_cols("l0_k", ev_w_in, 0, 8, [(512, 512)])
    plan_cols("l0_v", ev_w_in, 0, 8, [(1024, 512)])
    plan_cols("l0_vb", ev_w_in, 0, 8, [(2056, 512)])
    plan_cols("l0_u", ev_w_in, 0, 8, [(1544, 512)])
    plan_cols("l0_wo", ev_w_out, 0, 8, [(0, 512), (512, 512)])

    def plan_xattn(l):
        plan_cols(f"x{l}_q", x_wq, l * D, 8, [(0, 512)])
        plan_cols(f"x{l}_k", x_wkv, l * D, 8, [(0, 512)])
        plan_cols(f"x{l}_v", x_wkv, l * D, 8, [(512, 512)])
        plan_cols(f"x{l}_o", x_wo, l * 512, 4, [(0, 1024)])

    def plan_ffn2(name, W13, r13, W2, r2, ff, nparts):
        per = ff // 128 // nparts
        parts = []
        for q in range(nparts):
            pairs = []
            for (c0, n) in chunk_groups(q * per * 128, per, per=2):
                pi = ring.add_multi([(W13, r13, 8, c0, n), (W13, r13, 8, ff + c0, n)])
                pairs.append((pi, n))
            ncol = 512 if per * 512 <= RING_ELEMS else 256
            w2 = [(ring.add(W2, r2 + q * per * 128, per, c0, ncol), ncol) for c0 in range(0, D, ncol)]
            parts.append((pairs, w2, per))
        P[name] = parts

    plan_xattn(0)
    plan_ffn2("ffn0", ffn_w13, 0, ffn_w2, 0, FF_DENSE, 2)
    for nm, c0 in [("l1_q", 1536), ("l1_k", 2048), ("l1_v", 2560), ("l1_bg", 0), ("l1_cg", 512), ("l1_xi", 1024)]:
        plan_cols(nm, od_w_in, 0, 8, [(c0, 512)])
    plan_cols("l1_wo", od_w_out, 0, 8, [(0, 512), (512, 512)])
    plan_xattn(1)
    for e in range(NEXP):
        plan_ffn2(f"moe{e}", moe_w13, e * D, moe_w2, e * FF_EXP, FF_EXP, 4)

    setup_dma(ident[:], ident_d[:, :])
    setup_dma(tri[:], tri_d[:, :])
    setup_dma(gv[:].rearrange("p a b -> p (a b)"), gv_d[:, :])
    for e in (ACT, DVE, PE, POOL):
        e.wait((s_setup, 16 * setup_n[0]))
    DVE.mark(nc.vector.tensor_copy(tri_bf[:], tri[:]))
    DVE.mark(nc.vector.memset(ones_bf[:], 1.0))
    barrier()

    def rng(r, n=512):
        return slice(r * n, (r + 1) * n)

    def linear_fm(tiles, kc, rhs_fn, evac_fn, nr=NR, m0=0, pre_fn=None):
        m = m0
        for (ti, ncols) in tiles:
            wt = ring.get(ti)
            last = None
            for mm in range(ncols // 128):
                for r in range(nr):
                    b = ps_alloc()
                    b.buf.wr(PE)
                    if pre_fn is not None:
                        pre_fn(m, r)
                    rhs0 = rhs_fn(0, r)
                    n = rhs0.shape[-1]
                    for kk in range(kc):
                        ins = nc.tensor.matmul(b.t[:, 0:n], lhsT=wt[:, kk, mm * 128:(mm + 1) * 128],
                                               rhs=rhs_fn(kk, r), start=(kk == 0), stop=(kk == kc - 1))
                    ev = PE.mark(ins)
                    b.buf.wr_done(ev)
                    last = ev
                    evs = evac_fn(m, r, b)
                    ps_free(b, *evs)
                m += 1
            ring.release(ti, last)
        return m

    def linear_tm(tiles, kc, lhs_fn, ntiles, evac_fn):
        for ci, (ti, ncols) in enumerate(tiles):
            wt = ring.get(ti)
            last = None
            for t in range(ntiles):
                b = ps_alloc()
                b.buf.wr(PE)
                for kk in range(kc):
                    ins = nc.tensor.matmul(b.t[:, 0:ncols], lhsT=lhs_fn(kk, t), rhs=wt[:, kk, 0:ncols],
                                           start=(kk == 0), stop=(kk == kc - 1))
                ev = PE.mark(ins)
                b.buf.wr_done(ev)
                last = ev
                evs = evac_fn(ci, t, b, ncols)
                ps_free(b, *evs)
            ring.release(ti, last)

    alt = [0]

    def copy_evac(dst_ap, b, n=512, scale=None):
        alt[0] ^= 1
        if alt[0]:
            b.buf.rd(ACT)
            if scale is None:
                ev = ACT.mark(nc.scalar.copy(out=dst_ap, in_=b.t[:, 0:n]))
            else:
                ev = ACT.mark(nc.scalar.mul(out=dst_ap, in_=b.t[:, 0:n], mul=scale))
        else:
            b.buf.rd(DVE)
            if scale is None:
                ev = DVE.mark(nc.vector.tensor_copy(dst_ap, b.t[:, 0:n]))
            else:
                ev = DVE.mark(nc.vector.tensor_scalar(out=dst_ap, in0=b.t[:, 0:n], scalar1=scale, scalar2=None,
                                                       op0=ALU.mult))
        return [ev]

    def resid_evac(m, r, b):
        b.buf.rd(DVE)
        ev = DVE.mark(nc.vector.tensor_tensor(out=xT[:, m, rng(r)], in0=b.t[:, :], in1=xT[:, m, rng(r)], op=ALU.add))
        return [ev]

    def rmsnorm_fm(gidx, hT, st, hook=None, out_dt_tile=None):
        with ExitStack() as ls:
            sq = [sb(f"nsq{i}", [128, KC, 512], BF16, ls) for i in range(2)]
            lnb = [sb(f"nln{i}", [128, 512], F32, ls) for i in range(2)]
            rsb = [sb(f"nrs{i}", [128, 512], F32, ls) for i in range(2)]
            sqB, lnB, rsB = [Buf(), Buf()], [Buf(), Buf()], [Buf(), Buf()]

            def emit_sq(r):
                p = r % 2
                sqB[p].wr(ACT)
                ev = ACT.mark(nc.scalar.activation(out=sq[p][:, :, :], in_=xT[:, :, rng(r)], func=AF.Square))
                sqB[p].wr_done(ev)

            emit_sq(0)
            for r in range(NR):
                p = r % 2
                b = ps_alloc()
                b.buf.wr(PE)
                sqB[p].rd(PE)
                for c in range(KC):
                    ins = nc.tensor.matmul(b.t[:, :], lhsT=ones_bf[:, :], rhs=sq[p][:, c, :], start=(c == 0), stop=(c == KC - 1))
                ev = PE.mark(ins)
                sqB[p].rd_done(ev)
                b.buf.wr_done(ev)
                if r + 1 < NR:
                    emit_sq(r + 1)
                b.buf.rd(ACT)
                lnB[p].wr(ACT)
                ev = ACT.mark(nc.scalar.activation(out=lnb[p][:], in_=b.t[:, :], func=AF.Ln, bias=EPS, scale=1.0 / D))
                lnB[p].wr_done(ev)
                ps_free(b, ev)
                lnB[p].rd(ACT)
                rsB[p].wr(ACT)
                ev = ACT.mark(nc.scalar.activation(out=rsb[p][:], in_=lnb[p][:], func=AF.Exp, scale=-0.5))
                lnB[p].rd_done(ev)
                rsB[p].wr_done(ev)
                rsB[p].rd(DVE)
                for c in range(KC):
                    ev = DVE.mark(nc.vector.scalar_tensor_tensor(out=hT[:, c, rng(r)], in0=xT[:, c, rng(r)],
                                                                 scalar=gv[:, gidx, c:c + 1], in1=rsb[p][:],
                                                                 op0=ALU.mult, op1=ALU.mult))
                rsB[p].rd_done(ev)
                if hook is not None:
                    hook(r, rsb[p], rsB[p])
            barrier()

    def recip_act(dst, src_ps_ap, tmp, tmpB, dstB):
        tmpB.wr(ACT)
        ev = ACT.mark(nc.scalar.activation(out=tmp, in_=src_ps_ap, func=AF.Ln))
        tmpB.wr_done(ev)
        ev_ln = ev
        tmpB.rd(ACT)
        dstB.wr(ACT)
        ev = ACT.mark(nc.scalar.activation(out=dst, in_=tmp, func=AF.Exp, scale=-1.0))
        tmpB.rd_done(ev)
        dstB.wr_done(ev)
        return ev_ln, ev

    def gelu_evac(dst_ap, b, n, scr=None, scrB=None):
        b.buf.rd(ACT)
        ev = ACT.mark(nc.scalar.activation(out=dst_ap, in_=b.t[:, 0:n], func=AF.Gelu_apprx_tanh))
        return [ev]

    with ExitStack() as st:
        NSTG = 6
        stg = [sb(f"xstg{i}", [128, D], F32, st) for i in range(NSTG)]
        stg_sem = [sem(f"s_xstg{i}") for i in range(NSTG)]
        stgB = [Buf() for _ in range(NSTG)]
        memg = sb("memg_s", [128, D], F32, st)
        junk = sb("mjunk", [128, D], F32, st)
        mss = sb("mss", [128, 2], F32, st)
        nc.sync.dma_start(out=memg[:], in_=memg_d[:, :]).then_inc(s_misc, 16)
        misc_n = 1
        nload = [0] * NSTG

        def load_tile(src_rows, i):
            s = i % NSTG
            stgB[s].wr(SP)
            nc.sync.dma_start(out=stg[s][:], in_=src_rows).then_inc(stg_sem[s], 16)
            nload[s] += 1
            stgB[s].wr_done((stg_sem[s], 16 * nload[s]))
            return s

        srcs = [x_d[t * 128:(t + 1) * 128, :] for t in range(16)] + [mem_d[t * 128:(t + 1) * 128, :] for t in range(2)]
        for i, src in enumerate(srcs):
            s = load_tile(src, i)
            if i >= 16:
                mt = i - 16
                stgB[s].rd(ACT)
                ACT.wait((s_misc, 16))
                ev = ACT.mark(nc.scalar.activation(out=junk[:], in_=stg[s][:], func=AF.Square, accum_out=mss[:, mt:mt + 1]))
                ACT.wait(ev)
                ev = ACT.mark(nc.scalar.activation(out=mss[:, mt:mt + 1], in_=mss[:, mt:mt + 1], func=AF.Ln, bias=EPS, scale=1.0 / D))
                ACT.wait(ev)
                ev = ACT.mark(nc.scalar.activation(out=mss[:, mt:mt + 1], in_=mss[:, mt:mt + 1], func=AF.Exp, scale=-0.5))
                DVE.wait(ev)
                stgB[s].rd(DVE)
                ev = DVE.mark(nc.vector.scalar_tensor_tensor(out=stg[s][:], in0=stg[s][:], scalar=mss[:, mt:mt + 1],
                                                             in1=memg[:], op0=ALU.mult, op1=ALU.mult))
                stgB[s].wr_done(ev)
            for half in range(2):
                b = ps_alloc()
                b.buf.wr(PE)
                stgB[s].rd(PE)
                for cc in range(4):
                    c = half * 4 + cc
                    ins = nc.tensor.transpose(b.t[:, cc * 128:(cc + 1) * 128], stg[s][:, c * 128:(c + 1) * 128], ident[:, :])
                ev = PE.mark(ins)
                b.buf.wr_done(ev)
                stgB[s].rd_done(ev)
                src_v = b.t[:, :].rearrange("p (c n) -> p c n", c=4)
                if i < 16:
                    dst = xT[:, half * 4:half * 4 + 4, i * 128:(i + 1) * 128]
                else:
                    dst = memT[:, half * 4:half * 4 + 4, (i - 16) * 128:(i - 15) * 128]
                alt[0] ^= 1
                if alt[0]:
                    b.buf.rd(ACT)
                    ev = ACT.mark(nc.scalar.copy(out=dst, in_=src_v))
                else:
                    b.buf.rd(DVE)
                    ev = DVE.mark(nc.vector.tensor_copy(dst, src_v))
                ps_free(b, ev)
        barrier()

    def dump_dbg():
        dbg_d = nc.dram_tensor("dbg", [D, T], F32, kind="ExternalOutput").ap()
        barrier()
        nc.sync.dma_start(out=dbg_d.rearrange("(c p) t -> p c t", p=128), in_=xT[:]).then_inc(s_misc, 16)
        SP.wait((s_misc, 16 * 2))
        return nc

    if stop_after == "load":
        return dump_dbg()

    def xattn(l):
        with ExitStack() as st:
            hT = sb("hT", [128, KC, T], BF16, st)
            rmsnorm_fm(G_X0 if l == 0 else G_X1, hT, st)
            QxT = sb("QxT", [128, 4, T], BF16, st)
            KmT = sb("KmT", [128, 4, 256], BF16, st)
            Vm = sb("Vm", [128, 2, 512], BF16, st)
            NXE = 8
            E = [sb(f"xE{i}", [128, 512], BF16, st) for i in range(NXE)]
            EB = [Buf() for _ in range(NXE)]
            tmpA = [sb(f"xtmpA{i}", [128, 512], F32, st) for i in range(2)]
            tmpR = [sb(f"xtmpR{i}", [128, 512], F32, st) for i in range(2)]
            tA, tR = [Buf(), Buf()], [Buf(), Buf()]
            linear_fm(P[f"x{l}_q"], 8, lambda kk, r: hT[:, kk, rng(r)], lambda m, r, b: copy_evac(QxT[:, m, rng(r)], b))
            linear_fm(P[f"x{l}_k"], 8, lambda kk, r: memT[:, kk, :], lambda m, r, b: copy_evac(KmT[:, m, :], b, n=256), nr=1)
            linear_tm(P[f"x{l}_v"], 8, lambda kk, t: memT[:, kk, t * 128:(t + 1) * 128], 2,
                      lambda ci, t, b, n: copy_evac(Vm[:, t, :], b))
            barrier()
            sc = 128 ** -0.5
            state = {"ei": 0}

            def xs_emit(h, r):
                es_ = []
                for mt in range(2):
                    b = ps_alloc()
                    b.buf.wr(PE)
                    ev = PE.mark(nc.tensor.matmul(b.t[:, :], lhsT=KmT[:, h, mt * 128:(mt + 1) * 128], rhs=QxT[:, h, rng(r)],
                                                  start=True, stop=True))
                    b.buf.wr_done(ev)
                    e_i = state["ei"] % NXE
                    state["ei"] += 1
                    b.buf.rd(ACT)
                    EB[e_i].wr(ACT)
                    ev = ACT.mark(nc.scalar.activation(out=E[e_i][:], in_=b.t[:, :], func=AF.Exp, scale=sc))
                    EB[e_i].wr_done(ev)
                    ps_free(b, ev)
                    es_.append(e_i)
                return (h, r, es_)

            def xpv_emit(item):
                h, r, es_ = item
                num = ps_alloc()
                den = ps_alloc()
                num.buf.wr(PE)
                den.buf.wr(PE)
                for mt, e_i in enumerate(es_):
                    EB[e_i].rd(PE)
                    nc.tensor.matmul(num.t[:, :], lhsT=Vm[:, mt, h * 128:(h + 1) * 128], rhs=E[e_i][:], start=(mt == 0), stop=(mt == 1))
                    ev = PE.mark(nc.tensor.matmul(den.t[:, :], lhsT=ones_bf[:, :], rhs=E[e_i][:], start=(mt == 0), stop=(mt == 1)))
                    EB[e_i].rd_done(ev)
                num.buf.wr_done(ev)
                den.buf.wr_done(ev)
                den.buf.rd(ACT)
                state["pv"] = state.get("pv", 0) + 1
                p = state["pv"] % 2
                ev_ln, ev_r = recip_act(tmpR[p][:], den.t[:, :], tmpA[p][:], tA[p], tR[p])
                ps_free(den, ev_ln)
                num.buf.rd(DVE)
                tR[p].rd(DVE)
                ev = DVE.mark(nc.vector.tensor_tensor(out=QxT[:, h, rng(r)], in0=num.t[:, :], in1=tmpR[p][:], op=ALU.mult))
                tR[p].rd_done(ev)
                ps_free(num, ev)

            queue = []
            for h in range(4):
                for r in range(NR):
                    queue.append(xs_emit(h, r))
                    if len(queue) > 3:
                        xpv_emit(queue.pop(0))
            while queue:
                xpv_emit(queue.pop(0))
            barrier()
            linear_fm(P[f"x{l}_o"], 4, lambda kk, r: QxT[:, kk, rng(r)], resid_evac)
            barrier()

    def ffn_parts(parts, hT, hid, st_bufs, gate_b=None):
        sg, sgB, tg, tgB = st_bufs
        si = 0
        hid_ev = [None] * NR
        for (pairs, w2, per) in parts:
            j0 = 0
            for (pi, n) in pairs:
                wg, wu = ring.get(pi)
                nch = n // 128
                lastg = lastu = None
                for jj in range(nch):
                    for r in range(NR):
                        ba = ps_alloc()
                        ba.buf.wr(PE)
                        for kk in range(8):
                            ins = nc.tensor.matmul(ba.t[:, :], lhsT=wg[:, kk, jj * 128:(jj + 1) * 128], rhs=hT[:, kk, rng(r)],
                                                   start=(kk == 0), stop=(kk == 7))
                        lastg = PE.mark(ins)
                        ba.buf.wr_done(lastg)
                        bb = ps_alloc()
                        bb.buf.wr(PE)
                        for kk in range(8):
                            ins = nc.tensor.matmul(bb.t[:, :], lhsT=wu[:, kk, jj * 128:(jj + 1) * 128], rhs=hT[:, kk, rng(r)],
                                                   start=(kk == 0), stop=(kk == 7))
                        lastu = PE.mark(ins)
                        bb.buf.wr_done(lastu)
                        s = si % 2
                        si += 1
                        ba.buf.rd(ACT)
                        sgB[s].wr(ACT)
                        ev = ACT.mark(nc.scalar.activation(out=sg[s][:], in_=ba.t[:, :], func=AF.Silu))
                        sgB[s].wr_done(ev)
                        ps_free(ba, ev)
                        src = sg[s]
                        srcB = sgB[s]
                        if gate_b is not None:
                            sgB[s].rd(DVE)
                            tgB[s].wr(DVE)
                            ev = DVE.mark(nc.vector.tensor_tensor(out=tg[s][:], in0=sg[s][:], in1=gate_b[:, rng(r)], op=ALU.mult))
                            sgB[s].rd_done(ev)
                            tgB[s].wr_done(ev)
                            src = tg[s]
                            srcB = tgB[s]
                        srcB.rd(DVE)
                        bb.buf.rd(DVE)
                        ev = DVE.mark(nc.vector.tensor_tensor(out=hid[:, j0 + jj, rng(r)], in0=src[:], in1=bb.t[:, :], op=ALU.mult))
                        hid_ev[r] = ev
                        srcB.rd_done(ev)
                        ps_free(bb, ev)
                ring.release(pi, lastu)
                j0 += nch
            linear_fm(w2, per, lambda kk, r: hid[:, kk, rng(r)], resid_evac, pre_fn=lambda m, r: PE.wait(hid_ev[r]))
            DVE.wait(PE.now())

    def ffn_dense():
        with ExitStack() as st:
            hT = sb("hT", [128, KC, T], BF16, st)
            rmsnorm_fm(G_F0, hT, st)
            hid = sb("hid", [128, 11, T], BF16, st)
            sg = [sb(f"sg{i}", [128, 512], F32, st) for i in range(2)]
            ffn_parts(P["ffn0"], hT, hid, (sg, [Buf(), Buf()], None, None))
            barrier()

    def mixer0():
        with ExitStack() as st:
            hT = sb("hT", [128, KC, T], BF16, st)
            rmsnorm_fm(G_EV, hT, st)
            QT = sb("QT", [128, 4, T], BF16, st)
            wf = sb("wf", [128, 8, 8], BF16, st)
            bf8 = sb("bf8_s", [8, 1], F32, st)
            nbf8 = sb("nbf8", [8, 1], F32, st)
            sel72 = sb("sel72", [128, 8, 128], BF16, st)
            cq3 = sb("cq3", [128, T], BF16, st)
            cpos_tok = sb("cpos_tok", [128, 16, 8], F32, st)
            s_m0 = sem("s_m0")
            s_m0g = sem("s_m0g")
            nc.sync.dma_start(out=bf8[:], in_=bf8_d[:, :]).then_inc(s_m0, 16)
            nc.gpsimd.dma_start(out=wf[:], in_=ev_w_in[:, 1536:1544].rearrange("(k p) n -> p k n", p=128)).then_inc(s_m0g, 16)
            nc.gpsimd.dma_start(out=sel72[:].rearrange("p a b -> p (a b)"), in_=sel72_d[:, :]).then_inc(s_m0g, 16)
            for e_ in (ACT, DVE, PE):
                e_.wait((s_m0, 16), (s_m0g, 32))
            DVE.mark(nc.vector.tensor_scalar(out=nbf8[:], in0=bf8[:], scalar1=-1.0, scalar2=None, op0=ALU.mult))
            DVE.mark(nc.vector.memset(cq3[:], 0.0))
            barrier()

            with ExitStack() as st2:
                cpos = sb("cpos", [8, T], F32, st2)
                onesr = sb("onesr", [8, T], F32, st2)
                cres = sb("cres", [8, T], F32, st2)
                sp_ = sb("sp_", [8, T], F32, st2)
                etmp = sb("etmp", [8, 512], F32, st2)
                cmid = sb("cmid", [8, T], BF16, st2)
                clo = sb("clo", [8, T], BF16, st2)
                DVE.mark(nc.vector.memset(onesr[:], 1.0))
                for r in range(NR):
                    b = ps_alloc()
                    b.buf.wr(PE)
                    for kk in range(8):
                        ins = nc.tensor.matmul(b.t[0:8, :], lhsT=wf[:, kk, :], rhs=hT[:, kk, rng(r)], start=(kk == 0), stop=(kk == 7))
                    ev = PE.mark(ins)
                    b.buf.wr_done(ev)
                    b.buf.rd(ACT)
                    ACT.wait(ACT.now())
                    ev = ACT.mark(nc.scalar.activation(out=etmp[:], in_=b.t[0:8, :], func=AF.Exp, bias=nbf8[:, 0:1], scale=-1.0))
                    ps_free(b, ev)
                    ACT.wait(ev)
                    ev = ACT.mark(nc.scalar.activation(out=sp_[:, rng(r)], in_=etmp[:], func=AF.Ln, bias=1.0, scale=1.0))
                barrier()
                DVE.mark(nc.vector.tensor_tensor_scan(out=cpos[:], data0=onesr[:], data1=sp_[:], initial=0.0, op0=ALU.mult, op1=ALU.add))
                DVE.wait(DVE.now())
                DVE.mark(nc.vector.tensor_scalar(out=cres[:], in0=cpos[:], scalar1=-1.0, scalar2=None, op0=ALU.mult))
                DVE.wait(DVE.now())
                parts = [cq3[0:8, :], cmid[:], clo[:]]
                for i in range(3):
                    DVE.mark(nc.vector.tensor_copy(parts[i], cres[:]))
                    DVE.wait(DVE.now())
                    if i < 2:
                        DVE.mark(nc.vector.tensor_tensor(out=cres[:], in0=cres[:], in1=parts[i], op=ALU.subtract))
                        DVE.wait(DVE.now())
                SP.wait(DVE.now())
                nc.sync.dma_start(out=cq3[32:40, :], in_=cmid[:]).then_inc(s_m0, 16)
                nc.sync.dma_start(out=cq3[64:72, :], in_=clo[:]).then_inc(s_m0, 16)
                b = ps_alloc()
                b.buf.wr(PE)
                PE.wait(DVE.now())
                for t in range(16):
                    ins = nc.tensor.transpose(b.t[:, t * 8:(t + 1) * 8], cpos[:, t * 128:(t + 1) * 128], ident[0:8, 0:8])
                ev = PE.mark(ins)
                DVE.wait(ev)
                ev = DVE.mark(nc.vector.tensor_copy(cpos_tok[:].rearrange("p a b -> p (a b)"), b.t[:, 0:128]))
                ps_free(b, ev)
                for e_ in engs:
                    e_.wait((s_m0, 48))
                barrier()

            with ExitStack() as st2:
                KT = sb("KT", [128, 4, T], BF16, st2)
                Vt = sb("Vt", [128, 16, 512], BF16, st2)
                NE = 8
                E = [sb(f"fE{i}", [128, 512], BF16, st2) for i in range(NE)]
                EB = [Buf() for _ in range(NE)]
                tmpA = sb("ftmpA", [128, 512], F32, st2)
                tmpR = sb("ftmpR", [128, 512], F32, st2)
                tA, tR = Buf(), Buf()
                linear_fm(P["l0_q"], 8, lambda kk, r: hT[:, kk, rng(r)], lambda m, r, b: copy_evac(QT[:, m, rng(r)], b, scale=0.125))
                linear_fm(P["l0_k"], 8, lambda kk, r: hT[:, kk, rng(r)], lambda m, r, b: copy_evac(KT[:, m, rng(r)], b))
                linear_tm(P["l0_v"], 8, lambda kk, t: hT[:, kk, t * 128:(t + 1) * 128], 16,
                          lambda ci, t, b, n: copy_evac(Vt[:, t, :], b))
                barrier()
                state = {"ei": 0}
                COLT = True

                def s_emit(hp, i, r):
                    qlo = max(i * 128, r * 512)
                    n = (r + 1) * 512 - qlo
                    bs_ = [ps_alloc(), ps_alloc()]
                    for b in bs_:
                        b.buf.wr(PE)
                    for a in range(2):
                        nc.tensor.matmul(bs_[a].t[:, 0:n], lhsT=KT[a * 64:a * 64 + 64, hp, i * 128:(i + 1) * 128],
                                         rhs=QT[a * 64:a * 64 + 64, hp, qlo:qlo + n], start=True, stop=False)
                    for a in range(2):
                        ins = nc.tensor.matmul(bs_[a].t[:, 0:n], lhsT=sel72[:, 2 * hp + a, :], rhs=cq3[:, qlo:qlo + n], start=False, stop=True)
                    ev = PE.mark(ins)
                    eis = []
                    for a in range(2):
                        b = bs_[a]
                        b.buf.wr_done(ev)
                        e_i = state["ei"] % NE
                        state["ei"] += 1
                        b.buf.rd(ACT)
                        EB[e_i].wr(ACT)
                        ev2 = ACT.mark(nc.scalar.activation(out=E[e_i][:, 0:n], in_=b.t[:, 0:n], func=AF.Exp,
                                                            bias=cpos_tok[:, i, 2 * hp + a:2 * hp + a + 1], scale=1.0))
                        EB[e_i].wr_done(ev2)
                        ps_free(b, ev2)
                        if i * 128 >= r * 512:
                            DVE.wait(ev2)
                            ev3 = DVE.mark(nc.vector.tensor_tensor(out=E[e_i][:, 0:128], in0=E[e_i][:, 0:128], in1=tri_bf[:], op=ALU.mult))
                            EB[e_i].wr_done(ev3)
                        eis.append(e_i)
                    return (hp, i, r, eis, qlo, n)

                acc = {}

                def pv_emit(item):
                    hp, i, r, eis, qlo, n = item
                    last = 4 * r + 3
                    if i == 0:
                        acc["num"] = ps_alloc()
                        acc["den"] = ps_alloc()
                        acc["num"].buf.wr(PE)
                        acc["den"].buf.wr(PE)
                    num, den = acc["num"], acc["den"]
                    o0 = qlo - r * 512
                    for a in range(2):
                        EB[eis[a]].rd(PE)
                    for a in range(2):
                        rs = slice(a * 64, a * 64 + 64)
                        nc.tensor.matmul(num.t[rs, o0:o0 + n], lhsT=Vt[:, i, hp * 128 + a * 64:hp * 128 + a * 64 + 64], rhs=E[eis[a]][:, 0:n],
                                         start=(i == 0), stop=(i == last), tile_position=(0, a * 64))
                    for a in range(2):
                        rs = slice(a * 64, a * 64 + 64)
                        ins = nc.tensor.matmul(den.t[rs, o0:o0 + n], lhsT=ones_bf[:, 0:64], rhs=E[eis[a]][:, 0:n],
                                               start=(i == 0), stop=(i == last), tile_position=(0, a * 64))
                    ev = PE.mark(ins)
                    for a in range(2):
                        EB[eis[a]].rd_done(ev)
                    if i == last:
                        num.buf.wr_done(ev)
                        den.buf.wr_done(ev)
                        den.buf.rd(ACT)
                        ev_ln, ev_r = recip_act(tmpR[:], den.t[:, :], tmpA[:], tA, tR)
                        ps_free(den, ev_ln)
                        num.buf.rd(DVE)
                        tR.rd(DVE)
                        ev = DVE.mark(nc.vector.tensor_tensor(out=QT[:, hp, rng(r)], in0=num.t[:, :], in1=tmpR[:], op=ALU.mult))
                        tR.rd_done(ev)
                        ps_free(num, ev)

                for r in range(NR):
                    queue = []
                    for hp in range(4):
                        for i in range(4 * r + 4):
                            queue.append(s_emit(hp, i, r))
                            if len(queue) > 3:
                                pv_emit(queue.pop(0))
                    while queue:
                        pv_emit(queue.pop(0))
                barrier()

            with ExitStack() as st2:
                uT = sb("uT", [128, 4, T], BF16, st2)
                vtok = sb("vtok", [128, 16, 512], BF16, st2)
                gjunk = sb("gjunk", [128, 128], F32, st2)
                wsT_f = sb("wsT_f", [128, 512], F32, st2)
                wsT = sb("wsT_b", [128, 4, 128], BF16, st2)
                bs_f = sb("bs_f", [1, 512], F32, st2)
                bs_hi = sb("bs_hi", [1, 512], BF16, st2)
                bs_lo = sb("bs_lo", [1, 512], BF16, st2)
                bs_r = sb("bs_r", [1, 512], F32, st2)
                sgu = sb("sgu_s", [128, 512], F32, st2)
                s_m0b = sem("s_m0b")
                for dst, src in [(wsT_f[:], wsT_d[:, :]), (bs_f[:], bs_d[:, :]), (sgu[:], sgu_d[:, :])]:
                    nc.sync.dma_start(out=dst, in_=src).then_inc(s_m0b, 16)
                for e_ in (ACT, DVE, PE):
                    e_.wait((s_m0b, 48))
                for g in range(4):
                    DVE.mark(nc.vector.tensor_tensor(out=wsT[:, g, :], in0=wsT_f[:, g * 128:(g + 1) * 128], in1=tri[:], op=ALU.mult))
                DVE.mark(nc.vector.tensor_copy(bs_hi[:], bs_f[:]))
                DVE.wait(DVE.now())
                DVE.mark(nc.vector.tensor_tensor(out=bs_r[:], in0=bs_f[:], in1=bs_hi[:], op=ALU.subtract))
                DVE.wait(DVE.now())
                DVE.mark(nc.vector.tensor_copy(bs_lo[:], bs_r[:]))
                barrier()

                gss_all = sb("gss_all", [128, 64], F32, st2)

                def vb_evac(ci, t, b, n):
                    b.buf.rd(ACT)
                    ev = ACT.mark(nc.scalar.activation(out=vtok[:, t, :], in_=b.t[:, 0:512], func=AF.Gelu_apprx_tanh))
                    ACT.wait(ev)
                    for g in range(4):
                        ACT.mark(nc.scalar.activation(out=gjunk[:], in_=vtok[:, t, g * 128:(g + 1) * 128], func=AF.Square,
                                                      accum_out=gss_all[:, t * 4 + g:t * 4 + g + 1]))
                    return [ev]

                linear_tm(P["l0_vb"], 8, lambda kk, t: hT[:, kk, t * 128:(t + 1) * 128], 16, vb_evac)
                ACT.wait(ACT.now())
                ev = ACT.mark(nc.scalar.activation(out=gss_all[:], in_=gss_all[:], func=AF.Ln, bias=EPS, scale=1.0 / 128))
                ACT.wait(ev)
                ev = ACT.mark(nc.scalar.activation(out=gss_all[:], in_=gss_all[:], func=AF.Exp, scale=-0.5))
                DVE.wait(ev)
                for t in range(16):
                    for g in range(4):
                        DVE.mark(nc.vector.scalar_tensor_tensor(out=vtok[:, t, g * 128:(g + 1) * 128], in0=vtok[:, t, g * 128:(g + 1) * 128],
                                                                scalar=gss_all[:, t * 4 + g:t * 4 + g + 1], in1=sgu[:, g * 128:(g + 1) * 128],
                                                                op0=ALU.mult, op1=ALU.mult))
                linear_fm(P["l0_u"], 8, lambda kk, r: hT[:, kk, rng(r)],
                          lambda m, r, b: gelu_evac(uT[:, m, rng(r)], b, 512))
                barrier()
                for g in range(4):
                    for r in range(NR):
                        b = ps_alloc()
                        b.buf.wr(PE)
                        for j in range(4):
                            t = 4 * r + j
                            o = b.t[:, j * 128:(j + 1) * 128]
                            nc.tensor.matmul(o, lhsT=vtok[:, t, g * 128:(g + 1) * 128], rhs=wsT[:, g, :], start=True, stop=False)
                            nc.tensor.matmul(o, lhsT=ones_bf[0:1, :], rhs=bs_hi[0:1, g * 128:(g + 1) * 128], start=False, stop=False)
                            ins = nc.tensor.matmul(o, lhsT=ones_bf[0:1, :], rhs=bs_lo[0:1, g * 128:(g + 1) * 128], start=False, stop=True)
                        ev = PE.mark(ins)
                        b.buf.wr_done(ev)
                        b.buf.rd(DVE)
                        ev = DVE.mark(nc.vector.tensor_tensor(out=uT[:, g, rng(r)], in0=b.t[:, :], in1=uT[:, g, rng(r)], op=ALU.mult))
                        ps_free(b, ev)
                barrier()
                linear_fm(P["l0_wo"], 8, lambda kk, r: (QT[:, kk, rng(r)] if kk < 4 else uT[:, kk - 4, rng(r)]), resid_evac)
                barrier()

    def mixer1():
        with ExitStack() as st:
            hT = sb("hT", [128, KC, T], BF16, st)
            rmsnorm_fm(G_OD, hT, st)
            QT = sb("QT", [128, 4, T], BF16, st)
            convw = sb("convw_s", [128, 4, 3], F32, st)
            neglam = sb("neglam", [128, 1], F32, st)
            subln = sb("subln_s", [128, 1], F32, st)
            ebt = sb("ebt", [128, 8, 128], BF16, st)
            s_m1 = sem("s_m1")
            with ExitStack() as st2:
                lamv = sb("lamv_s", [128, 4, 64], F32, st2)
                lprod = sb("lprod", [128, 2, 64], F32, st2)
                lsum = sb("lsum", [128, 2], F32, st2)
                btab = sb("btab_s", [128, 8, 128], F32, st2)
                rel31 = sb("rel31_s", [128, 4], F32, st2)
                nm1 = 0
                for dst, src in [(convw[:].rearrange("p a b -> p (a b)"), convw_d[:, :]), (lamv[:].rearrange("p a b -> p (a b)"), lamv_d[:, :]),
                                 (subln[:], subln_d[:, :]), (btab[:].rearrange("p a b -> p (a b)"), btab_d[:, :]), (rel31[:], rel31_d[:, :])]:
                    nc.sync.dma_start(out=dst, in_=src).then_inc(s_m1, 16)
                    nm1 += 1
                for e_ in (ACT, DVE):
                    e_.wait((s_m1, 16 * nm1))
                DVE.mark(nc.vector.tensor_tensor(out=lprod[:, 0, :], in0=lamv[:, 0, :], in1=lamv[:, 1, :], op=ALU.mult))
                DVE.mark(nc.vector.tensor_tensor(out=lprod[:, 1, :], in0=lamv[:, 2, :], in1=lamv[:, 3, :], op=ALU.mult))
                DVE.wait(DVE.now())
                DVE.mark(nc.vector.reduce_sum(out=lsum[:], in_=lprod[:], axis=AX.X))
                ACT.wait(DVE.now())
                ev = ACT.mark(nc.scalar.activation(out=lsum[:], in_=lsum[:], func=AF.Exp))
                DVE.wait(ev)
                DVE.mark(nc.vector.tensor_tensor(out=neglam[:], in0=lsum[:, 1:2], in1=lsum[:, 0:1], op=ALU.subtract))
                DVE.wait(DVE.now())
                DVE.mark(nc.vector.tensor_scalar(out=neglam[:], in0=neglam[:], scalar1=-LAM_INIT, scalar2=None, op0=ALU.add))
                DVE.mark(nc.vector.tensor_scalar(out=subln[:], in0=subln[:], scalar1=(1.0 - LAM_INIT), scalar2=None, op0=ALU.mult))
                for h in range(4):
                    for w in range(2):
                        DVE.mark(nc.vector.tensor_scalar(out=btab[:, h * 2 + w, :], in0=btab[:, h * 2 + w, :], scalar1=rel31[:, h:h + 1], scalar2=None,
                                                         op0=ALU.subtract))
                ACT.wait(DVE.now())
                ev = ACT.mark(nc.scalar.activation(out=btab[:].rearrange("p a b -> p (a b)"), in_=btab[:].rearrange("p a b -> p (a b)"), func=AF.Exp))
                DVE.wait(ev)
                for h in range(4):
                    DVE.mark(nc.vector.tensor_tensor(out=ebt[:, h * 2, :], in0=btab[:, h * 2, :], in1=tri[:], op=ALU.mult))
                    DVE.mark(nc.vector.tensor_copy(ebt[:, h * 2 + 1, :], btab[:, h * 2 + 1, :]))
                barrier()

            with ExitStack() as st2:
                KT = sb("KT", [128, 4, T], BF16, st2)
                Vt = sb("Vt", [128, 16, 512], BF16, st2)
                NE = 8
                E = [sb(f"dE{i}", [128, 512], BF16, st2) for i in range(NE)]
                EB = [Buf() for _ in range(NE)]
                tmpA = sb("dtmpA", [128, 512], F32, st2)
                rr = [sb(f"drr{i}", [128, 512], F32, st2) for i in range(2)]
                tA, rB = Buf(), [Buf(), Buf()]
                o_t = sb("do_t", [128, 512], F32, st2)
                o_sq = sb("do_sq", [128, 512], BF16, st2)
                oB, sqB = Buf(), Buf()
                linear_fm(P["l1_q"], 8, lambda kk, r: hT[:, kk, rng(r)], lambda m, r, b: copy_evac(QT[:, m, rng(r)], b, scale=0.125))
                linear_fm(P["l1_k"], 8, lambda kk, r: hT[:, kk, rng(r)], lambda m, r, b: copy_evac(KT[:, m, rng(r)], b))
                linear_tm(P["l1_v"], 8, lambda kk, t: hT[:, kk, t * 128:(t + 1) * 128], 16,
                          lambda ci, t, b, n: copy_evac(Vt[:, t, :], b))
                barrier()
                state = {"ei": 0}

                def s_emit(h, i, r):
                    qlo = max(i * 128, r * 512)
                    n = (r + 1) * 512 - qlo
                    eis = []
                    for br in range(2):
                        ps_ = slice(br * 64, br * 64 + 64)
                        b = ps_alloc()
                        b.buf.wr(PE)
                        ev = PE.mark(nc.tensor.matmul(b.t[:, 0:n], lhsT=KT[ps_, h, i * 128:(i + 1) * 128], rhs=QT[ps_, h, qlo:qlo + n],
                                                      start=True, stop=True))
                        b.buf.wr_done(ev)
                        e_i = state["ei"] % NE
                        state["ei"] += 1
                        b.buf.rd(ACT)
                        EB[e_i].wr(ACT)
                        ev = ACT.mark(nc.scalar.activation(out=E[e_i][:, 0:n], in_=b.t[:, 0:n], func=AF.Exp))
                        EB[e_i].wr_done(ev)
                        ps_free(b, ev)
                        fixes = []
                        if i * 128 >= r * 512:
                            fixes.append((0, 0))
                            if n > 128:
                                fixes.append((128, 1))
                        elif i == 4 * r - 1:
                            fixes.append((0, 1))
                        for (c0, w) in fixes:
                            DVE.wait(EB[e_i].w)
                            ev = DVE.mark(nc.vector.tensor_tensor(out=E[e_i][:, c0:c0 + 128], in0=E[e_i][:, c0:c0 + 128], in1=ebt[:, h * 2 + w, :], op=ALU.mult))
                            EB[e_i].wr_done(ev)
                        eis.append(e_i)
                    return (h, i, r, eis, qlo, n)

                acc = {}

                def pv_emit(item):
                    h, i, r, eis, qlo, n = item
                    last = 4 * r + 3
                    if i == 0:
                        for nm in ("n1", "d1", "n2", "d2"):
                            acc[nm] = ps_alloc()
                            acc[nm].buf.wr(PE)
                    o0 = qlo - r * 512
                    for br in range(2):
                        e_i = eis[br]
                        num, den = acc[f"n{br + 1}"], acc[f"d{br + 1}"]
                        EB[e_i].rd(PE)
                        nc.tensor.matmul(num.t[:, o0:o0 + n], lhsT=Vt[:, i, h * 128:(h + 1) * 128], rhs=E[e_i][:, 0:n], start=(i == 0), stop=(i == last))
                        ev = PE.mark(nc.tensor.matmul(den.t[:, o0:o0 + n], lhsT=ones_bf[:, :], rhs=E[e_i][:, 0:n], start=(i == 0), stop=(i == last)))
                        EB[e_i].rd_done(ev)
                    if i == last:
                        for nm in ("n1", "d1", "n2", "d2"):
                            acc[nm].buf.wr_done(ev)
                        for br in range(2):
                            den = acc[f"d{br + 1}"]
                            den.buf.rd(ACT)
                            ev_ln, ev_r = recip_act(rr[br][:], den.t[:, :], tmpA[:], tA, rB[br])
                            ps_free(den, ev_ln)
                        for br in range(2):
                            num = acc[f"n{br + 1}"]
                            num.buf.rd(DVE)
                            rB[br].rd(DVE)
                            ev = DVE.mark(nc.vector.tensor_tensor(out=rr[br][:], in0=num.t[:, :], in1=rr[br][:], op=ALU.mult))
                            rB[br].wr_done(ev)
                            ps_free(num, ev)
                        rB[0].rd(DVE)
                        rB[1].rd(DVE)
                        oB.wr(DVE)
                        ev = DVE.mark(nc.vector.scalar_tensor_tensor(out=o_t[:], in0=rr[1][:], scalar=neglam[:, 0:1], in1=rr[0][:], op0=ALU.mult, op1=ALU.add))
                        rB[0].rd_done(ev)
                        rB[1].rd_done(ev)
                        oB.wr_done(ev)
                        dq.append([DLY_A, (lambda h=h, r=r: stage2a(h, r))])

                dq = []
                st2bank = {}

                def stage2a(h, r):
                    oB.rd(ACT)
                    sqB.wr(ACT)
                    ev = ACT.mark(nc.scalar.activation(out=o_sq[:], in_=o_t[:], func=AF.Square))
                    oB.rd_done(ev)
                    sqB.wr_done(ev)
                    b = ps_alloc()
                    b.buf.wr(PE)
                    sqB.rd(PE)
                    ev = PE.mark(nc.tensor.matmul(b.t[:, :], lhsT=ones_bf[:, :], rhs=o_sq[:], start=True, stop=True))
                    sqB.rd_done(ev)
                    b.buf.wr_done(ev)
                    st2bank[(h, r)] = b
                    dq.append([DLY_B, (lambda h=h, r=r: stage2b(h, r))])

                def stage2b(h, r):
                    b = st2bank.pop((h, r))
                    b.buf.rd(ACT)
                    tA.wr(ACT)
                    ev = ACT.mark(nc.scalar.activation(out=tmpA[:], in_=b.t[:, :], func=AF.Ln, bias=EPS, scale=1.0 / 128))
                    tA.wr_done(ev)
                    ps_free(b, ev)
                    ACT.wait(ev)
                    rB[0].wr(ACT)
                    ev = ACT.mark(nc.scalar.activation(out=rr[0][:], in_=tmpA[:], func=AF.Exp, scale=-0.5))
                    tA.rd_done(ev)
                    rB[0].wr_done(ev)
                    rB[0].rd(DVE)
                    oB.rd(DVE)
                    ev = DVE.mark(nc.vector.scalar_tensor_tensor(out=QT[:, h, rng(r)], in0=o_t[:], scalar=subln[:, 0:1], in1=rr[0][:], op0=ALU.mult, op1=ALU.mult))
                    rB[0].rd_done(ev)
                    oB.rd_done(ev)

                def tick(flush=False):
                    while True:
                        progressed = False
                        for ent in list(dq):
                            ent[0] -= 1
                        ready = [ent for ent in dq if ent[0] <= 0]
                        for ent in ready:
                            dq.remove(ent)
                            ent[1]()
                            progressed = True
                        if not flush or not dq:
                            break

                for r in range(NR):
                    queue = []
                    for h in range(4):
                        for i in range(4 * r + 4):
                            queue.append(s_emit(h, i, r))
                            tick()
                            if len(queue) > 2:
                                pv_emit(queue.pop(0))
                    while queue:
                        tick()
                        pv_emit(queue.pop(0))
                    tick(flush=True)
                barrier()

            with ExitStack() as st2:
                bgT = sb("bgT", [128, 4, T], BF16, st2)
                cgT = sb("cgT", [128, 4, T], BF16, st2)
                xiT = sb("xiT", [128, 4, T], BF16, st2)
                linear_fm(P["l1_bg"], 8, lambda kk, r: hT[:, kk, rng(r)], lambda m, r, b: copy_evac(bgT[:, m, rng(r)], b))
                linear_fm(P["l1_cg"], 8, lambda kk, r: hT[:, kk, rng(r)], lambda m, r, b: copy_evac(cgT[:, m, rng(r)], b))
                linear_fm(P["l1_xi"], 8, lambda kk, r: hT[:, kk, rng(r)], lambda m, r, b: copy_evac(xiT[:, m, rng(r)], b))
                barrier()
                ev1 = []
                for c in range(4):
                    ev1.append(DVE.mark(nc.vector.tensor_tensor(out=cgT[:, c, :], in0=cgT[:, c, :], in1=xiT[:, c, :], op=ALU.mult)))
                ev2 = []
                for c in range(4):
                    ACT.wait(ev1[c])
                    ev2.append(ACT.mark(nc.scalar.mul(out=xiT[:, c, :], in_=cgT[:, c, :], mul=convw[:, c, 2:3])))
                for c in range(4):
                    DVE.wait(ev2[c])
                    DVE.mark(nc.vector.scalar_tensor_tensor(out=xiT[:, c, 1:T], in0=cgT[:, c, 0:T - 1], scalar=convw[:, c, 1:2], in1=xiT[:, c, 1:T],
                                                            op0=ALU.mult, op1=ALU.add))
                    DVE.wait(DVE.now())
                    DVE.mark(nc.vector.scalar_tensor_tensor(out=xiT[:, c, 2:T], in0=cgT[:, c, 0:T - 2], scalar=convw[:, c, 0:1], in1=xiT[:, c, 2:T],
                                                            op0=ALU.mult, op1=ALU.add))
                    DVE.wait(DVE.now())
                    DVE.mark(nc.vector.tensor_tensor(out=bgT[:, c, :], in0=bgT[:, c, :], in1=xiT[:, c, :], op=ALU.mult))
                barrier()
                linear_fm(P["l1_wo"], 8, lambda kk, r: (bgT[:, kk, rng(r)] if kk < 4 else QT[:, kk - 4, rng(r)]), resid_evac)
                barrier()

    def moe():
        with ExitStack() as st:
            hT = sb("hT", [128, KC, T], BF16, st)
            wr = sb("wr_s", [128, 8, 8], F32, st)
            gwr = sb("gwr", [128, 8, 8], F32, st)
            selE = sb("selE", [8, 8, 128], F32, st)
            z = sb("z", [128, 16, 8], F32, st)
            m8 = sb("m8", [128, 16, 8], F32, st)
            negv1 = sb("negv1", [128, 16], F32, st)
            msk = sb("msk", [128, 16, 8], F32, st)
            ez = sb("ez", [128, 16, 8], F32, st)
            gden = sb("gden", [128, 16], F32, st)
            gT = sb("gT", [8, T], F32, st)
            rtmp = sb("rtmp", [128, 128], F32, st)
            s_mo = sem("s_mo")
            nc.sync.dma_start(out=wr[:].rearrange("p a b -> p (a b)"), in_=wr_d[:, :]).then_inc(s_mo, 16)
            nc.sync.dma_start(out=selE[:].rearrange("p a b -> p (a b)"), in_=sel_d[:, :]).then_inc(s_mo, 16)
            DVE.wait((s_mo, 32))
            PE.wait((s_mo, 32))
            for c in range(8):
                DVE.mark(nc.vector.tensor_scalar(out=gwr[:, c, :], in0=wr[:, c, :], scalar1=gv[:, G_F1, c:c + 1], scalar2=None, op0=ALU.mult))

            def hook(r, rs_tile, rs_buf):
                b = ps_alloc()
                b.buf.wr(PE)
                rs_buf.rd(PE)
                for j in range(4):
                    ins = nc.tensor.transpose(b.t[:, j * 128:(j + 1) * 128], rs_tile[:, j * 128:(j + 1) * 128], ident[:, :])
                ev = PE.mark(ins)
                rs_buf.rd_done(ev)
                b.buf.wr_done(ev)
                b.buf.rd(DVE)
                src = b.t[:, :].rearrange("p (j n) -> p j n", j=4)[:, :, 0:1]
                ev_copy = DVE.mark(nc.vector.tensor_copy(rstd_tok[:, 4 * r:4 * r + 4].rearrange("p (j o) -> p j o", o=1), src))
                ps_free(b, ev_copy)
                PE.wait(ev_gwr)
                b = ps_alloc()
                b.buf.wr(PE)
                for c in range(8):
                    ins = nc.tensor.matmul(b.t[0:8, :], lhsT=gwr[:, c, :], rhs=xT[:, c, rng(r)], start=(c == 0), stop=(c == 7))
                ev = PE.mark(ins)
                b.buf.wr_done(ev)
                b.buf.rd(ACT)
                ev_z = ACT.mark(nc.scalar.copy(out=gT[:, rng(r)], in_=b.t[0:8, :]))
                ps_free(b, ev_z)
                b2 = ps_alloc()
                b2.buf.wr(PE)
                PE.wait(ev_z)
                for j in range(4):
                    t = 4 * r + j
                    ins = nc.tensor.transpose(b2.t[:, j * 8:(j + 1) * 8], gT[:, t * 128:(t + 1) * 128], ident[0:8, 0:8])
                ev = PE.mark(ins)
                b2.buf.wr_done(ev)
                b2.buf.rd(DVE)
                DVE.wait(ev_copy)
                for j in range(4):
                    t = 4 * r + j
                    ev = DVE.mark(nc.vector.tensor_scalar(out=z[:, t, :], in0=b2.t[:, j * 8:(j + 1) * 8], scalar1=rstd_tok[:, t:t + 1], scalar2=None, op0=ALU.mult))
                ps_free(b2, ev)

            ev_gwr = DVE.now()
            rmsnorm_fm(G_F1, hT, st, hook=hook)
            DVE.wait(DVE.now())
            for t in range(16):
                DVE.mark(nc.vector.max(out=m8[:, t, :], in_=z[:, t, :]))
            DVE.wait(DVE.now())
            DVE.mark(nc.vector.tensor_scalar(out=negv1[:].rearrange("p (t o) -> p t o", o=1), in0=m8[:, :, 0:1], scalar1=-1.0, scalar2=None, op0=ALU.mult))
            DVE.mark(nc.vector.tensor_tensor(out=msk[:], in0=z[:], in1=m8[:, :, 1:2].to_broadcast([128, 16, 8]), op=ALU.is_ge))
            DVE.mark(nc.vector.tensor_tensor(out=ez[:], in0=z[:], in1=m8[:, :, 0:1].to_broadcast([128, 16, 8]), op=ALU.subtract))
            ACT.wait(DVE.now())
            ev = ACT.mark(nc.scalar.activation(out=ez[:].rearrange("p a b -> p (a b)"), in_=ez[:].rearrange("p a b -> p (a b)"), func=AF.Exp))
            DVE.wait(ev)
            DVE.wait(DVE.now())
            DVE.mark(nc.vector.tensor_tensor(out=ez[:].rearrange("p a b -> p (a b)"), in0=ez[:].rearrange("p a b -> p (a b)"),
                                             in1=msk[:].rearrange("p a b -> p (a b)"), op=ALU.mult))
            DVE.wait(DVE.now())
            DVE.mark(nc.vector.reduce_sum(out=gden[:], in_=ez[:], axis=AX.X))
            DVE.wait(DVE.now())
            DVE.mark(nc.vector.reciprocal(gden[:], gden[:]))
            DVE.wait(DVE.now())
            DVE.mark(nc.vector.tensor_tensor(out=ez[:], in0=ez[:], in1=gden[:].rearrange("p (t o) -> p t o", o=1).to_broadcast([128, 16, 8]), op=ALU.mult))
            PE.wait(DVE.now())
            for r in range(NR):
                b = ps_alloc()
                b.buf.wr(PE)
                for j in range(4):
                    ins = nc.tensor.transpose(b.t[0:8, j * 128:(j + 1) * 128], ez[:, 4 * r + j, :], ident[:, :])
                ev = PE.mark(ins)
                b.buf.wr_done(ev)
                b.buf.rd(DVE)
                ev = DVE.mark(nc.vector.tensor_copy(gT[:, rng(r)], b.t[0:8, :]))
                ps_free(b, ev)
            barrier()
            hid = sb("hid", [128, 7, T], BF16, st)
            sg = [sb(f"sg{i}", [128, 512], F32, st) for i in range(2)]
            tg = [sb(f"tg{i}", [128, 512], F32, st) for i in range(2)]
            gate_b = [sb(f"gate_b{i}", [128, T], F32, st) for i in range(2)]
            gbB = [Buf(), Buf()]
            bufs = (sg, [Buf(), Buf()], tg, [Buf(), Buf()])
            for e in range(NEXP):
                gb = gate_b[e % 2]
                for r in range(NR):
                    b = ps_alloc()
                    b.buf.wr(PE)
                    ev = PE.mark(nc.tensor.matmul(b.t[:, :], lhsT=selE[:, e, :], rhs=gT[:, rng(r)], start=True, stop=True))
                    b.buf.wr_done(ev)
                    b.buf.rd(ACT)
                    if r == 0:
                        gbB[e % 2].wr(ACT)
                    ev = ACT.mark(nc.scalar.copy(out=gb[:, rng(r)], in_=b.t[:, :]))
                    ps_free(b, ev)
                gbB[e % 2].wr_done(ev)
                gbB[e % 2].rd(DVE)
                ffn_parts(P[f"moe{e}"], hT, hid, bufs, gate_b=gb)
                gbB[e % 2].rd_done(DVE.now())
            barrier()

    def final_out():
        with ExitStack() as st:
            yT = [sb(f"yT{i}", [128, KC, 512], F32, st) for i in range(2)]
            sq = [sb(f"fsq{i}", [128, KC, 512], BF16, st) for i in range(2)]
            lnb = [sb(f"fln{i}", [128, 512], F32, st) for i in range(2)]
            rsb = [sb(f"frs{i}", [128, 512], F32, st) for i in range(2)]
            yB, sqB, lnB, rsB = [Buf(), Buf()], [Buf(), Buf()], [Buf(), Buf()], [Buf(), Buf()]
            NO = 3
            ost = [sb(f"ost{i}", [128, D], F32, st) for i in range(NO)]
            osem = [sem(f"s_ost{i}") for i in range(NO)]
            ocnt = [0] * NO
            oB = [Buf() for _ in range(NO)]
            oi = 0

            def emit_sq(r):
                p = r % 2
                sqB[p].wr(ACT)
                ev = ACT.mark(nc.scalar.activation(out=sq[p][:, :, :], in_=xT[:, :, rng(r)], func=AF.Square))
                sqB[p].wr_done(ev)

            emit_sq(0)
            for r in range(NR):
                p = r % 2
                b = ps_alloc()
                b.buf.wr(PE)
                sqB[p].rd(PE)
                for c in range(KC):
                    ins = nc.tensor.matmul(b.t[:, :], lhsT=ones_bf[:, :], rhs=sq[p][:, c, :], start=(c == 0), stop=(c == KC - 1))
                ev = PE.mark(ins)
                sqB[p].rd_done(ev)
                b.buf.wr_done(ev)
                if r + 1 < NR:
                    emit_sq(r + 1)
                b.buf.rd(ACT)
                lnB[p].wr(ACT)
                ev = ACT.mark(nc.scalar.activation(out=lnb[p][:], in_=b.t[:, :], func=AF.Ln, bias=EPS, scale=1.0 / D))
                lnB[p].wr_done(ev)
                ps_free(b, ev)
                lnB[p].rd(ACT)
                rsB[p].wr(ACT)
                ev = ACT.mark(nc.scalar.activation(out=rsb[p][:], in_=lnb[p][:], func=AF.Exp, scale=-0.5))
                lnB[p].rd_done(ev)
                rsB[p].wr_done(ev)
                rsB[p].rd(DVE)
                yB[p].wr(DVE)
                for c in range(KC):
                    ev = DVE.mark(nc.vector.scalar_tensor_tensor(out=yT[p][:, c, :], in0=xT[:, c, rng(r)], scalar=gv[:, G_FIN, c:c + 1], in1=rsb[p][:],
                                                                 op0=ALU.mult, op1=ALU.mult))
                rsB[p].rd_done(ev)
                yB[p].wr_done(ev)
                for j in range(4):
                    s = oi % NO
                    oi += 1
                    oB[s].wr(ACT)
                    oB[s].wr(DVE)
                    evs = []
                    for half in range(2):
                        b = ps_alloc()
                        b.buf.wr(PE)
                        yB[p].rd(PE)
                        for cc in range(4):
                            c = half * 4 + cc
                            ins = nc.tensor.transpose(b.t[:, cc * 128:(cc + 1) * 128], yT[p][:, c, j * 128:(j + 1) * 128], ident[:, :])
                        ev = PE.mark(ins)
                        yB[p].rd_done(ev)
                        b.buf.wr_done(ev)
                        evs += copy_evac(ost[s][:, half * 512:(half + 1) * 512], b)
                        ps_free(b, evs[-1])
                    SP.wait(*evs)
                    t = 4 * r + j
                    nc.sync.dma_start(out=out_d[t * 128:(t + 1) * 128, :], in_=ost[s][:]).then_inc(osem[s], 16)
                    ocnt[s] += 1
                    oB[s].wr_done(None)
                    oB[s].reads = [(osem[s], 16 * ocnt[s])]
            SP.wait(*[(osem[s], 16 * ocnt[s]) for s in range(NO) if ocnt[s]])

    phases = [("mixer0", mixer0), ("xattn0", lambda: xattn(0)), ("ffn0", ffn_dense),
              ("mixer1", mixer1), ("xattn1", lambda: xattn(1)), ("moe", moe)]
    for name, fn in phases:
        fn()
        if stop_after == name:
            return dump_dbg()
    final_out()
    return nc


def _t5_bucket(dist):
    n = np.maximum(dist, 0)
    nf = np.maximum(n, 1).astype(np.float32)
    large = 16 + (np.log(nf / np.float32(16)) / np.float32(math.log(128 / 16)) * np.float32(16)).astype(np.int32)
    large = np.minimum(large, 31)
    return np.where(n < 16, n, large)


def make_in_maps(inputs):
    f = lambda a: np.ascontiguousarray(np.asarray(a, dtype=np.float32))
    g = {k: np.asarray(v) for k, v in inputs.items()}
    gains = np.stack([g["ev_norm"][0], g["x_norm"][0], g["ffn_norm"][0], g["od_norm"][0], g["x_norm"][1], g["ffn_norm"][1],
                      g["final_norm"]], 0)
    gv = gains.reshape(7, 8, 128).transpose(2, 0, 1).reshape(128, 56)
    kk = np.arange(128)[:, None]
    qq = np.arange(128)[None, :]
    tri = (qq >= kk).astype(np.float32)
    bt = np.zeros((128, 4, 2, 128), np.float32)
    for w in range(2):
        bidx = _t5_bucket(qq - kk + 128 * w)
        bt[:, :, w, :] = np.transpose(g["rel_bias"][bidx], (0, 2, 1))
    sel = np.zeros((8, 8, 128), np.float32)
    for e in range(8):
        sel[e, e, :] = 1.0
    sel72 = np.zeros((128, 8, 128), np.float32)
    for e in range(8):
        for o in (0, 32, 64):
            sel72[o + e, e, :] = 1.0
    common = {
        "ident": np.eye(128, dtype=np.float32),
        "tri": tri,
        "gv": f(gv),
        "memg": f(np.broadcast_to(g["mem_norm"][None, :], (128, D))),
        "bf8": f(g["ev_b_f"][0].reshape(8, 1)),
        "sgu": f(np.broadcast_to(g["ev_sgu_norm"][0].reshape(1, 512), (128, 512))),
        "wsT": f(g["ev_w_s"][0].transpose(2, 0, 1).reshape(128, 512)),
        "bs": f(g["ev_b_s"][0].reshape(1, 512)),
        "convw": f(g["od_conv_w"][0].reshape(3, 4, 128).transpose(2, 1, 0).reshape(128, 12)),
        "lamv": f(np.broadcast_to(np.concatenate([g["od_lam_q1"][0], g["od_lam_k1"][0], g["od_lam_q2"][0], g["od_lam_k2"][0]])[None, :], (128, 256))),
        "subln": f(g["od_subln"][0].reshape(128, 1)),
        "btab": f(bt.reshape(128, 1024)),
        "rel31": f(np.broadcast_to(g["rel_bias"][31][None, :], (128, 4))),
        "wr": f(g["moe_router"][0].reshape(8, 128, 8).transpose(1, 0, 2).reshape(128, 64)),
        "sel": f(sel.reshape(8, 1024)),
        "sel72": f(sel72.reshape(128, 1024)),
        "ev_w_in": f(g["ev_w_in"][0]), "ev_w_out": f(g["ev_w_out"][0]),
        "ffn_w13": f(g["ffn_w13"][0]), "ffn_w2": f(g["ffn_w2"][0]),
        "od_w_in": f(g["od_w_in"][0]), "od_w_out": f(g["od_w_out"][0]),
        "moe_w13": f(g["moe_w13"][0].reshape(NEXP * D, 2 * FF_EXP)),
        "moe_w2": f(g["moe_w2"][0].reshape(NEXP * FF_EXP, D)),
        "x_wq": f(g["x_wq"].reshape(2 * D, 512)), "x_wkv": f(g["x_wkv"].reshape(2 * D, D)),
        "x_wo": f(g["x_wo"].reshape(2 * 512, D)),
    }
    maps = []
    for c in range(8):
        m = dict(common)
        m["x"] = f(g["x"][c])
        m["mem"] = f(g["mem"][c])
        maps.append(m)
    return maps


def kernel(**inputs):
    nc = build_nc()
    in_maps = make_in_maps(inputs)
    res = run_bass_kernel_spmd(nc, in_maps, core_ids=list(range(8)))
    return np.stack([np.asarray(r["out"], dtype=np.float32) for r in res.results], 0)
```

```python
import math
from contextlib import ExitStack
import numpy as np
import concourse.bass as bass
import concourse.mybir as mybir
from concourse.bass_utils import run_bass_kernel_spmd

F32 = mybir.dt.float32
BF16 = mybir.dt.bfloat16
AF = mybir.ActivationFunctionType
ALU = mybir.AluOpType
AX = mybir.AxisListType

T = 2048
NR = 4
D = 1024
KC = 8
EPS = 1e-6
FF_DENSE = 2816
FF_EXP = 3584
NEXP = 8
LAM_INIT = 0.8 - 0.6 * math.exp(-0.3 * 1)
RING_SLOTS = 4
RING_ELEMS = 4096
DLY_A = 2
DLY_B = 1
G_EV, G_X0, G_F0, G_OD, G_X1, G_F1, G_FIN = range(7)


class Eng:
    def __init__(self, raw, sem, name):
        self.raw, self.sem, self.name = raw, sem, name
        self.n = 0
        self.waited = {}
        self.last = None
        self.last_marked = True

    def wait(self, *evs):
        for ev in evs:
            if ev is None:
                continue
            if isinstance(ev, (list, tuple)) and ev and isinstance(ev[0], (list, tuple)):
                self.wait(*ev)
                continue
            sem, val = ev
            key = id(sem)
            if self.waited.get(key, 0) >= val:
                continue
            self.raw.wait_ge(sem, val)
            self.waited[key] = val

    def mark(self, inst):
        self.n += 1
        inst.then_inc(self.sem, 1)
        self.last = inst
        self.last_marked = True
        ev = (self.sem, self.n)
        return ev

    def nomark(self, inst):
        self.last = inst
        self.last_marked = False

    def now(self):
        if not self.last_marked and self.last is not None:
            return self.mark(self.last)
        if self.n == 0:
            return None
        return (self.sem, self.n)


class Buf:
    def __init__(self):
        self.w = None
        self.reads = []

    def rd(self, eng):
        eng.wait(self.w)

    def rd_done(self, ev):
        self.reads.append(ev)

    def wr(self, eng):
        eng.wait(self.w, *self.reads)

    def wr_done(self, ev):
        self.w = ev
        self.reads = []


class K:
    pass


def build_nc(stop_after=None, dbg=False):
    nc = bass.Bass("TRN2", target_bir_lowering=False)
    k = K()
    k.nc = nc
    es = ExitStack()
    k.es = es

    def din(name, shape):
        return nc.dram_tensor(name, list(shape), F32, kind="ExternalInput").ap()

    x_d = din("x", [T, D])
    mem_d = din("mem", [256, D])
    ident_d = din("ident", [128, 128])
    tri_d = din("tri", [128, 128])
    gv_d = din("gv", [128, 7 * 8])
    memg_d = din("memg", [128, D])
    bf8_d = din("bf8", [8, 1])
    sgu_d = din("sgu", [128, 512])
    wsT_d = din("wsT", [128, 512])
    bs_d = din("bs", [1, 512])
    convw_d = din("convw", [128, 12])
    lamv_d = din("lamv", [128, 256])
    subln_d = din("subln", [128, 1])
    btab_d = din("btab", [128, 4 * 2 * 128])
    rel31_d = din("rel31", [128, 4])
    wr_d = din("wr", [128, 64])
    sel_d = din("sel", [8, 8 * 128])
    sel72_d = din("sel72", [128, 8 * 128])
    ev_w_in = din("ev_w_in", [D, 2568])
    ev_w_out = din("ev_w_out", [D, D])
    ffn_w13 = din("ffn_w13", [D, 2 * FF_DENSE])
    ffn_w2 = din("ffn_w2", [FF_DENSE, D])
    od_w_in = din("od_w_in", [D, 3072])
    od_w_out = din("od_w_out", [D, D])
    moe_w13 = din("moe_w13", [NEXP * D, 2 * FF_EXP])
    moe_w2 = din("moe_w2", [NEXP * FF_EXP, D])
    x_wq = din("x_wq", [2 * D, 512])
    x_wkv = din("x_wkv", [2 * D, D])
    x_wo = din("x_wo", [2 * 512, D])
    out_d = nc.dram_tensor("out", [T, D], F32, kind="ExternalOutput").ap()

    uniq = [0]

    def sb(name, shape, dt=F32, stack=None):
        uniq[0] += 1
        return (stack or es).enter_context(nc.sbuf_tensor(f"sb{uniq[0]}_{name}", list(shape), dt))

    def sem(name):
        uniq[0] += 1
        return es.enter_context(nc.semaphore(f"{name}_{uniq[0]}"))

    PE = Eng(nc.tensor, sem("s_pe"), "pe")
    ACT = Eng(nc.scalar, sem("s_act"), "act")
    DVE = Eng(nc.vector, sem("s_dve"), "dve")
    POOL = Eng(nc.gpsimd, sem("s_pool"), "pool")
    SP = Eng(nc.sync, sem("s_sp"), "sp")
    engs = [PE, ACT, DVE, POOL, SP]
    dma_pending = []

    def barrier():
        evs = [e.now() for e in engs] + list(dma_pending)
        for e in engs:
            e.wait(*evs)
        del dma_pending[:]

    xT = sb("xT", [128, KC, T])
    memT = sb("memT", [128, KC, 256], BF16)
    ident = sb("ident_s", [128, 128])
    tri = sb("tri_s", [128, 128])
    tri_bf = sb("tri_bf", [128, 128], BF16)
    ones_bf = sb("ones_bf", [128, 128], BF16)
    gv = sb("gv_s", [128, 7, 8])
    rstd_tok = sb("rstd_tok", [128, 16])
    ring_t = [sb(f"ring{i}", [128, RING_ELEMS], BF16) for i in range(RING_SLOTS)]
    ring_sem = [sem(f"s_ring{i}") for i in range(RING_SLOTS)]
    ps_banks = [es.enter_context(nc.psum_tensor(f"ps{i}", [128, 512], F32)) for i in range(8)]
    s_setup = sem("s_setup")
    s_misc = sem("s_misc")
    setup_n = [0]

    def setup_dma(dst, src, eng=None):
        (eng or nc.sync).dma_start(out=dst, in_=src).then_inc(s_setup, 16)
        setup_n[0] += 1

    class Bank:
        def __init__(self, t, i):
            self.t, self.i = t, i
            self.buf = Buf()
            self.held = False
            self.stamp = 0
    banks = [Bank(t, i) for i, t in enumerate(ps_banks)]
    bank_clock = [0]

    def ps_alloc():
        free = [b for b in banks if not b.held]
        assert free, "no free PSUM bank"
        b = min(free, key=lambda b: b.stamp)
        b.held = True
        return b

    def ps_free(b, *evs):
        for ev in evs:
            b.buf.rd_done(ev)
        b.held = False
        bank_clock[0] += 1
        b.stamp = bank_clock[0]

    class Ring:
        def __init__(self):
            self.plan = []
            self.load_ev = {}
            self.released = {}
            self.issued = 0
            self.pos = 0
            self.loads = [0] * RING_SLOTS

        def add(self, W, r0, kc, c0, n):
            return self.add_multi([(W, r0, kc, c0, n)])

        def add_multi(self, parts):
            ent = []
            off = 0
            for (W, r0, kc, c0, n) in parts:
                src = W[r0:r0 + kc * 128, c0:c0 + n].rearrange("(k p) n -> p k n", p=128)
                ent.append((src, kc, n, off))
                off += kc * n
            assert off <= RING_ELEMS, off
            self.plan.append(ent)
            return len(self.plan) - 1

        def _try_issue(self):
            while self.issued < len(self.plan) and self.issued < self.pos + RING_SLOTS:
                j = self.issued
                s = j % RING_SLOTS
                if j >= RING_SLOTS:
                    if (j - RING_SLOTS) not in self.released:
                        break
                    POOL.wait(self.released[j - RING_SLOTS])
                for (src, kc, n, off) in self.plan[j]:
                    dst = ring_t[s][:, off:off + kc * n].rearrange("p (k n) -> p k n", k=kc)
                    nc.gpsimd.dma_start(out=dst, in_=src).then_inc(ring_sem[s], 16)
                    self.loads[s] += 1
                self.load_ev[j] = (ring_sem[s], 16 * self.loads[s])
                self.issued += 1

        def get(self, j):
            assert j == self.pos, (j, self.pos)
            self._try_issue()
            assert j in self.load_ev
            PE.wait(self.load_ev[j])
            self.pos += 1
            views = [ring_t[j % RING_SLOTS][:, off:off + kc * n].rearrange("p (k n) -> p k n", k=kc)
                     for (src, kc, n, off) in self.plan[j]]
            return views[0] if len(views) == 1 else views

        def release(self, j, ev):
            self.released[j] = ev
            self._try_issue()

    ring = Ring()

    P = {}

    def plan_cols(name, W, r0, kc, cols):
        P[name] = [(ring.add(W, r0, kc, c0, n), n) for (c0, n) in cols]

    def chunk_groups(c_start, nchunks, per=4):
        out = []
        j = 0
        while j < nchunks:
            n = min(per, nchunks - j)
            out.append((c_start + j * 128, n * 128))
            j += n
        return out

    plan_cols("l0_q", ev_w_in, 0, 8, [(0, 512)])
    plan_cols("l0_k", ev_w_in, 0, 8, [(512, 512)])
    plan_cols("l0_v", ev_w_in, 0, 8, [(1024, 512)])
    plan_cols("l0_vb", ev_w_in, 0, 8, [(2056, 512)])
    plan_cols("l0_u", ev_w_in, 0, 8, [(1544, 512)])
    plan_cols("l0_wo", ev_w_out, 0, 8, [(0, 512), (512, 512)])

    def plan_xattn(l):
        plan_cols(f"x{l}_q", x_wq, l * D, 8, [(0, 512)])
        plan_cols(f"x{l}_k", x_wkv, l * D, 8, [(0, 512)])
        plan_cols(f"x{l}_v", x_wkv, l * D, 8, [(512, 512)])
        plan_cols(f"x{l}_o", x_wo, l * 512, 4, [(0, 1024)])

    def plan_ffn2(name, W13, r13, W2, r2, ff, nparts):
        per = ff // 128 // nparts
        parts = []
        for q in range(nparts):
            pairs = []
            for (c0, n) in chunk_groups(q * per * 128, per, per=2):
                pi = ring.add_multi([(W13, r13, 8, c0, n), (W13, r13, 8, ff + c0, n)])
                pairs.append((pi, n))
            ncol = 512 if per * 512 <= RING_ELEMS else 256
            w2 = [(ring.add(W2, r2 + q * per * 128, per, c0, ncol), ncol) for c0 in range(0, D, ncol)]
            parts.append((pairs, w2, per))
        P[name] = parts

    plan_xattn(0)
    plan_ffn2("ffn0", ffn_w13, 0, ffn_w2, 0, FF_DENSE, 2)
    for nm, c0 in [("l1_q", 1536), ("l1_k", 2048), ("l1_v", 2560), ("l1_bg", 0), ("l1_cg", 512), ("l1_xi", 1024)]:
        plan_cols(nm, od_w_in, 0, 8, [(c0, 512)])
    plan_cols("l1_wo", od_w_out, 0, 8, [(0, 512), (512, 512)])
    plan_xattn(1)
    for e in range(NEXP):
        plan_ffn2(f"moe{e}", moe_w13, e * D, moe_w2, e * FF_EXP, FF_EXP, 4)

    setup_dma(ident[:], ident_d[:, :])
    setup_dma(tri[:], tri_d[:, :])
    setup_dma(gv[:].rearrange("p a b -> p (a b)"), gv_d[:, :])
    for e in (ACT, DVE, PE, POOL):
        e.wait((s_setup, 16 * setup_n[0]))
    DVE.mark(nc.vector.tensor_copy(tri_bf[:], tri[:]))
    DVE.mark(nc.vector.memset(ones_bf[:], 1.0))
    barrier()

    def rng(r, n=512):
        return slice(r * n, (r + 1) * n)

    def linear_fm(tiles, kc, rhs_fn, evac_fn, nr=NR, m0=0, pre_fn=None):
        m = m0
        for (ti, ncols) in tiles:
            wt = ring.get(ti)
            last = None
            for mm in range(ncols // 128):
                for r in range(nr):
                    b = ps_alloc()
                    b.buf.wr(PE)
                    if pre_fn is not None:
                        pre_fn(m, r)
                    rhs0 = rhs_fn(0, r)
                    n = rhs0.shape[-1]
                    for kk in range(kc):
                        ins = nc.tensor.matmul(b.t[:, 0:n], lhsT=wt[:, kk, mm * 128:(mm + 1) * 128],
                                               rhs=rhs_fn(kk, r), start=(kk == 0), stop=(kk == kc - 1))
                    ev = PE.mark(ins)
                    b.buf.wr_done(ev)
                    last = ev
                    evs = evac_fn(m, r, b)
                    ps_free(b, *evs)
                m += 1
            ring.release(ti, last)
        return m

    def linear_tm(tiles, kc, lhs_fn, ntiles, evac_fn):
        for ci, (ti, ncols) in enumerate(tiles):
            wt = ring.get(ti)
            last = None
            for t in range(ntiles):
                b = ps_alloc()
                b.buf.wr(PE)
                for kk in range(kc):
                    ins = nc.tensor.matmul(b.t[:, 0:ncols], lhsT=lhs_fn(kk, t), rhs=wt[:, kk, 0:ncols],
                                           start=(kk == 0), stop=(kk == kc - 1))
                ev = PE.mark(ins)
                b.buf.wr_done(ev)
                last = ev
                evs = evac_fn(ci, t, b, ncols)
                ps_free(b, *evs)
            ring.release(ti, last)

    alt = [0]

    def copy_evac(dst_ap, b, n=512, scale=None):
        alt[0] ^= 1
        if alt[0]:
            b.buf.rd(ACT)
            if scale is None:
                ev = ACT.mark(nc.scalar.copy(out=dst_ap, in_=b.t[:, 0:n]))
            else:
                ev = ACT.mark(nc.scalar.mul(out=dst_ap, in_=b.t[:, 0:n], mul=scale))
        else:
            b.buf.rd(DVE)
            if scale is None:
                ev = DVE.mark(nc.vector.tensor_copy(dst_ap, b.t[:, 0:n]))
            else:
                ev = DVE.mark(nc.vector.tensor_scalar(out=dst_ap, in0=b.t[:, 0:n], scalar1=scale, scalar2=None,
                                                       op0=ALU.mult))
        return [ev]

    def resid_evac(m, r, b):
        b.buf.rd(DVE)
        ev = DVE.mark(nc.vector.tensor_tensor(out=xT[:, m, rng(r)], in0=b.t[:, :], in1=xT[:, m, rng(r)], op=ALU.add))
        return [ev]

    def rmsnorm_fm(gidx, hT, st, hook=None, out_dt_tile=None):
        with ExitStack() as ls:
            sq = [sb(f"nsq{i}", [128, KC, 512], BF16, ls) for i in range(2)]
            lnb = [sb(f"nln{i}", [128, 512], F32, ls) for i in range(2)]
            rsb = [sb(f"nrs{i}", [128, 512], F32, ls) for i in range(2)]
            sqB, lnB, rsB = [Buf(), Buf()], [Buf(), Buf()], [Buf(), Buf()]

            def emit_sq(r):
                p = r % 2
                sqB[p].wr(ACT)
                ev = ACT.mark(nc.scalar.activation(out=sq[p][:, :, :], in_=xT[:, :, rng(r)], func=AF.Square))
                sqB[p].wr_done(ev)

            emit_sq(0)
            for r in range(NR):
                p = r % 2
                b = ps_alloc()
                b.buf.wr(PE)
                sqB[p].rd(PE)
                for c in range(KC):
                    ins = nc.tensor.matmul(b.t[:, :], lhsT=ones_bf[:, :], rhs=sq[p][:, c, :], start=(c == 0), stop=(c == KC - 1))
                ev = PE.mark(ins)
                sqB[p].rd_done(ev)
                b.buf.wr_done(ev)
                if r + 1 < NR:
                    emit_sq(r + 1)
                b.buf.rd(ACT)
                lnB[p].wr(ACT)
                ev = ACT.mark(nc.scalar.activation(out=lnb[p][:], in_=b.t[:, :], func=AF.Ln, bias=EPS, scale=1.0 / D))
                lnB[p].wr_done(ev)
                ps_free(b, ev)
                lnB[p].rd(ACT)
                rsB[p].wr(ACT)
                ev = ACT.mark(nc.scalar.activation(out=rsb[p][:], in_=lnb[p][:], func=AF.Exp, scale=-0.5))
                lnB[p].rd_done(ev)
                rsB[p].wr_done(ev)
                rsB[p].rd(DVE)
                for c in range(KC):
                    ev = DVE.mark(nc.vector.scalar_tensor_tensor(out=hT[:, c, rng(r)], in0=xT[:, c, rng(r)],
                                                                 scalar=gv[:, gidx, c:c + 1], in1=rsb[p][:],
                                                                 op0=ALU.mult, op1=ALU.mult))
                rsB[p].rd_done(ev)
                if hook is not None:
                    hook(r, rsb[p], rsB[p])
            barrier()

    def recip_act(dst, src_ps_ap, tmp, tmpB, dstB):
        tmpB.wr(ACT)
        ev = ACT.mark(nc.scalar.activation(out=tmp, in_=src_ps_ap, func=AF.Ln))
        tmpB.wr_done(ev)
        ev_ln = ev
        tmpB.rd(ACT)
        dstB.wr(ACT)
        ev = ACT.mark(nc.scalar.activation(out=dst, in_=tmp, func=AF.Exp, scale=-1.0))
        tmpB.rd_done(ev)
        dstB.wr_done(ev)
        return ev_ln, ev

    def gelu_evac(dst_ap, b, n, scr=None, scrB=None):
        b.buf.rd(ACT)
        ev = ACT.mark(nc.scalar.activation(out=dst_ap, in_=b.t[:, 0:n], func=AF.Gelu_apprx_tanh))
        return [ev]

    with ExitStack() as st:
        NSTG = 6
        stg = [sb(f"xstg{i}", [128, D], F32, st) for i in range(NSTG)]
        stg_sem = [sem(f"s_xstg{i}") for i in range(NSTG)]
        stgB = [Buf() for _ in range(NSTG)]
        memg = sb("memg_s", [128, D], F32, st)
        junk = sb("mjunk", [128, D], F32, st)
        mss = sb("mss", [128, 2], F32, st)
        nc.sync.dma_start(out=memg[:], in_=memg_d[:, :]).then_inc(s_misc, 16)
        misc_n = 1
        nload = [0] * NSTG

        def load_tile(src_rows, i):
            s = i % NSTG
            stgB[s].wr(SP)
            nc.sync.dma_start(out=stg[s][:], in_=src_rows).then_inc(stg_sem[s], 16)
            nload[s] += 1
            stgB[s].wr_done((stg_sem[s], 16 * nload[s]))
            return s

        srcs = [x_d[t * 128:(t + 1) * 128, :] for t in range(16)] + [mem_d[t * 128:(t + 1) * 128, :] for t in range(2)]
        for i, src in enumerate(srcs):
            s = load_tile(src, i)
            if i >= 16:
                mt = i - 16
                stgB[s].rd(ACT)
                ACT.wait((s_misc, 16))
                ev = ACT.mark(nc.scalar.activation(out=junk[:], in_=stg[s][:], func=AF.Square, accum_out=mss[:, mt:mt + 1]))
                ACT.wait(ev)
                ev = ACT.mark(nc.scalar.activation(out=mss[:, mt:mt + 1], in_=mss[:, mt:mt + 1], func=AF.Ln, bias=EPS, scale=1.0 / D))
                ACT.wait(ev)
                ev = ACT.mark(nc.scalar.activation(out=mss[:, mt:mt + 1], in_=mss[:, mt:mt + 1], func=AF.Exp, scale=-0.5))
                DVE.wait(ev)
                stgB[s].rd(DVE)
                ev = DVE.mark(nc.vector.scalar_tensor_tensor(out=stg[s][:], in0=stg[s][:], scalar=mss[:, mt:mt + 1],
                                                             in1=memg[:], op0=ALU.mult, op1=ALU.mult))
                stgB[s].wr_done(ev)
            for half in range(2):
                b = ps_alloc()
                b.buf.wr(PE)
                stgB[s].rd(PE)
                for cc in range(4):
                    c = half * 4 + cc
                    ins = nc.tensor.transpose(b.t[:, cc * 128:(cc + 1) * 128], stg[s][:, c * 128:(c + 1) * 128], ident[:, :])
                ev = PE.mark(ins)
                b.buf.wr_done(ev)
                stgB[s].rd_done(ev)
                src_v = b.t[:, :].rearrange("p (c n) -> p c n", c=4)
                if i < 16:
                    dst = xT[:, half * 4:half * 4 + 4, i * 128:(i + 1) * 128]
                else:
                    dst = memT[:, half * 4:half * 4 + 4, (i - 16) * 128:(i - 15) * 128]
                alt[0] ^= 1
                if alt[0]:
                    b.buf.rd(ACT)
                    ev = ACT.mark(nc.scalar.copy(out=dst, in_=src_v))
                else:
                    b.buf.rd(DVE)
                    ev = DVE.mark(nc.vector.tensor_copy(dst, src_v))
                ps_free(b, ev)
        barrier()

    def dump_dbg():
        dbg_d = nc.dram_tensor("dbg", [D, T], F32, kind="ExternalOutput").ap()
        barrier()
        nc.sync.dma_start(out=dbg_d.rearrange("(c p) t -> p c t", p=128), in_=xT[:]).then_inc(s_misc, 16)
        SP.wait((s_misc, 16 * 2))
        return nc

    if stop_after == "load":
        return dump_dbg()

    def xattn(l):
        with ExitStack() as st:
            hT = sb("hT", [128, KC, T], BF16, st)
            rmsnorm_fm(G_X0 if l == 0 else G_X1, hT, st)
            QxT = sb("QxT", [128, 4, T], BF16, st)
            KmT = sb("KmT", [128, 4, 256], BF16, st)
            Vm = sb("Vm", [128, 2, 512], BF16, st)
            NXE = 8
            E = [sb(f"xE{i}", [128, 512], BF16, st) for i in range(NXE)]
            EB = [Buf() for _ in range(NXE)]
            tmpA = [sb(f"xtmpA{i}", [128, 512], F32, st) for i in range(2)]
            tmpR = [sb(f"xtmpR{i}", [128, 512], F32, st) for i in range(2)]
            tA, tR = [Buf(), Buf()], [Buf(), Buf()]
            linear_fm(P[f"x{l}_q"], 8, lambda kk, r: hT[:, kk, rng(r)], lambda m, r, b: copy_evac(QxT[:, m, rng(r)], b))
            linear_fm(P[f"x{l}_k"], 8, lambda kk, r: memT[:, kk, :], lambda m, r, b: copy_evac(KmT[:, m, :], b, n=256), nr=1)
            linear_tm(P[f"x{l}_v"], 8, lambda kk, t: memT[:, kk, t * 128:(t + 1) * 128], 2,
                      lambda ci, t, b, n: copy_evac(Vm[:, t, :], b))
            barrier()
            sc = 128 ** -0.5
            state = {"ei": 0}

            def xs_emit(h, r):
                es_ = []
                for mt in range(2):
                    b = ps_alloc()
                    b.buf.wr(PE)
                    ev = PE.mark(nc.tensor.matmul(b.t[:, :], lhsT=KmT[:, h, mt * 128:(mt + 1) * 128], rhs=QxT[:, h, rng(r)],
                                                  start=True, stop=True))
                    b.buf.wr_done(ev)
                    e_i = state["ei"] % NXE
                    state["ei"] += 1
                    b.buf.rd(ACT)
                    EB[e_i].wr(ACT)
                    ev = ACT.mark(nc.scalar.activation(out=E[e_i][:], in_=b.t[:, :], func=AF.Exp, scale=sc))
                    EB[e_i].wr_done(ev)
                    ps_free(b, ev)
                    es_.append(e_i)
                return (h, r, es_)

            def xpv_emit(item):
                h, r, es_ = item
                num = ps_alloc()
                den = ps_alloc()
                num.buf.wr(PE)
                den.buf.wr(PE)
                for mt, e_i in enumerate(es_):
                    EB[e_i].rd(PE)
                    nc.tensor.matmul(num.t[:, :], lhsT=Vm[:, mt, h * 128:(h + 1) * 128], rhs=E[e_i][:], start=(mt == 0), stop=(mt == 1))
                    ev = PE.mark(nc.tensor.matmul(den.t[:, :], lhsT=ones_bf[:, :], rhs=E[e_i][:], start=(mt == 0), stop=(mt == 1)))
                    EB[e_i].rd_done(ev)
                num.buf.wr_done(ev)
                den.buf.wr_done(ev)
                den.buf.rd(ACT)
                state["pv"] = state.get("pv", 0) + 1
                p = state["pv"] % 2
                ev_ln, ev_r = recip_act(tmpR[p][:], den.t[:, :], tmpA[p][:], tA[p], tR[p])
                ps_free(den, ev_ln)
                num.buf.rd(DVE)
                tR[p].rd(DVE)
                ev = DVE.mark(nc.vector.tensor_tensor(out=QxT[:, h, rng(r)], in0=num.t[:, :], in1=tmpR[p][:], op=ALU.mult))
                tR[p].rd_done(ev)
                ps_free(num, ev)

            queue = []
            for h in range(4):
                for r in range(NR):
                    queue.append(xs_emit(h, r))
                    if len(queue) > 3:
                        xpv_emit(queue.pop(0))
            while queue:
                xpv_emit(queue.pop(0))
            barrier()
            linear_fm(P[f"x{l}_o"], 4, lambda kk, r: QxT[:, kk, rng(r)], resid_evac)
            barrier()

    def ffn_parts(parts, hT, hid, st_bufs, gate_b=None):
        sg, sgB, tg, tgB = st_bufs
        si = 0
        hid_ev = [None] * NR
        for (pairs, w2, per) in parts:
            j0 = 0
            for (pi, n) in pairs:
                wg, wu = ring.get(pi)
                nch = n // 128
                lastg = lastu = None
                for jj in range(nch):
                    for r in range(NR):
                        ba = ps_alloc()
                        ba.buf.wr(PE)
                        for kk in range(8):
                            ins = nc.tensor.matmul(ba.t[:, :], lhsT=wg[:, kk, jj * 128:(jj + 1) * 128], rhs=hT[:, kk, rng(r)],
                                                   start=(kk == 0), stop=(kk == 7))
                        lastg = PE.mark(ins)
                        ba.buf.wr_done(lastg)
                        bb = ps_alloc()
                        bb.buf.wr(PE)
                        for kk in range(8):
                            ins = nc.tensor.matmul(bb.t[:, :], lhsT=wu[:, kk, jj * 128:(jj + 1) * 128], rhs=hT[:, kk, rng(r)],
                                                   start=(kk == 0), stop=(kk == 7))
                        lastu = PE.mark(ins)
                        bb.buf.wr_done(lastu)
                        s = si % 2
                        si += 1
                        ba.buf.rd(ACT)
                        sgB[s].wr(ACT)
                        ev = ACT.mark(nc.scalar.activation(out=sg[s][:], in_=ba.t[:, :], func=AF.Silu))
                        sgB[s].wr_done(ev)
                        ps_free(ba, ev)
                        src = sg[s]
                        srcB = sgB[s]
                        if gate_b is not None:
                            sgB[s].rd(DVE)
                            tgB[s].wr(DVE)
                            ev = DVE.mark(nc.vector.tensor_tensor(out=tg[s][:], in0=sg[s][:], in1=gate_b[:, rng(r)], op=ALU.mult))
                            sgB[s].rd_done(ev)
                            tgB[s].wr_done(ev)
                            src = tg[s]
                            srcB = tgB[s]
                        srcB.rd(DVE)
                        bb.buf.rd(DVE)
                        ev = DVE.mark(nc.vector.tensor_tensor(out=hid[:, j0 + jj, rng(r)], in0=src[:], in1=bb.t[:, :], op=ALU.mult))
                        hid_ev[r] = ev
                        srcB.rd_done(ev)
                        ps_free(bb, ev)
                ring.release(pi, lastu)
                j0 += nch
            linear_fm(w2, per, lambda kk, r: hid[:, kk, rng(r)], resid_evac, pre_fn=lambda m, r: PE.wait(hid_ev[r]))
            DVE.wait(PE.now())

    def ffn_dense():
        with ExitStack() as st:
            hT = sb("hT", [128, KC, T], BF16, st)
            rmsnorm_fm(G_F0, hT, st)
            hid = sb("hid", [128, 11, T], BF16, st)
            sg = [sb(f"sg{i}", [128, 512], F32, st) for i in range(2)]
            ffn_parts(P["ffn0"], hT, hid, (sg, [Buf(), Buf()], None, None))
            barrier()

    def mixer0():
        with ExitStack() as st:
            hT = sb("hT", [128, KC, T], BF16, st)
            rmsnorm_fm(G_EV, hT, st)
            QT = sb("QT", [128, 4, T], BF16, st)
            wf = sb("wf", [128, 8, 8], BF16, st)
            bf8 = sb("bf8_s", [8, 1], F32, st)
            nbf8 = sb("nbf8", [8, 1], F32, st)
            sel72 = sb("sel72", [128, 8, 128], BF16, st)
            cq3 = sb("cq3", [128, T], BF16, st)
            cpos_tok = sb("cpos_tok", [128, 16, 8], F32, st)
            s_m0 = sem("s_m0")
            s_m0g = sem("s_m0g")
            nc.sync.dma_start(out=bf8[:], in_=bf8_d[:, :]).then_inc(s_m0, 16)
            nc.gpsimd.dma_start(out=wf[:], in_=ev_w_in[:, 1536:1544].rearrange("(k p) n -> p k n", p=128)).then_inc(s_m0g, 16)
            nc.gpsimd.dma_start(out=sel72[:].rearrange("p a b -> p (a b)"), in_=sel72_d[:, :]).then_inc(s_m0g, 16)
            for e_ in (ACT, DVE, PE):
                e_.wait((s_m0, 16), (s_m0g, 32))
            DVE.mark(nc.vector.tensor_scalar(out=nbf8[:], in0=bf8[:], scalar1=-1.0, scalar2=None, op0=ALU.mult))
            DVE.mark(nc.vector.memset(cq3[:], 0.0))
            barrier()

            with ExitStack() as st2:
                cpos = sb("cpos", [8, T], F32, st2)
                onesr = sb("onesr", [8, T], F32, st2)
                cres = sb("cres", [8, T], F32, st2)
                sp_ = sb("sp_", [8, T], F32, st2)
                etmp = sb("etmp", [8, 512], F32, st2)
                cmid = sb("cmid", [8, T], BF16, st2)
                clo = sb("clo", [8, T], BF16, st2)
                DVE.mark(nc.vector.memset(onesr[:], 1.0))
                for r in range(NR):
                    b = ps_alloc()
                    b.buf.wr(PE)
                    for kk in range(8):
                        ins = nc.tensor.matmul(b.t[0:8, :], lhsT=wf[:, kk, :], rhs=hT[:, kk, rng(r)], start=(kk == 0), stop=(kk == 7))
                    ev = PE.mark(ins)
                    b.buf.wr_done(ev)
                    b.buf.rd(ACT)
                    ACT.wait(ACT.now())
                    ev = ACT.mark(nc.scalar.activation(out=etmp[:], in_=b.t[0:8, :], func=AF.Exp, bias=nbf8[:, 0:1], scale=-1.0))
                    ps_free(b, ev)
                    ACT.wait(ev)
                    ev = ACT.mark(nc.scalar.activation(out=sp_[:, rng(r)], in_=etmp[:], func=AF.Ln, bias=1.0, scale=1.0))
                barrier()
                DVE.mark(nc.vector.tensor_tensor_scan(out=cpos[:], data0=onesr[:], data1=sp_[:], initial=0.0, op0=ALU.mult, op1=ALU.add))
                DVE.wait(DVE.now())
                DVE.mark(nc.vector.tensor_scalar(out=cres[:], in0=cpos[:], scalar1=-1.0, scalar2=None, op0=ALU.mult))
                DVE.wait(DVE.now())
                parts = [cq3[0:8, :], cmid[:], clo[:]]
                for i in range(3):
                    DVE.mark(nc.vector.tensor_copy(parts[i], cres[:]))
                    DVE.wait(DVE.now())
                    if i < 2:
                        DVE.mark(nc.vector.tensor_tensor(out=cres[:], in0=cres[:], in1=parts[i], op=ALU.subtract))
                        DVE.wait(DVE.now())
                SP.wait(DVE.now())
                nc.sync.dma_start(out=cq3[32:40, :], in_=cmid[:]).then_inc(s_m0, 16)
                nc.sync.dma_start(out=cq3[64:72, :], in_=clo[:]).then_inc(s_m0, 16)
                b = ps_alloc()
                b.buf.wr(PE)
                PE.wait(DVE.now())
                for t in range(16):
                    ins = nc.tensor.transpose(b.t[:, t * 8:(t + 1) * 8], cpos[:, t * 128:(t + 1) * 128], ident[0:8, 0:8])
                ev = PE.mark(ins)
                DVE.wait(ev)
                ev = DVE.mark(nc.vector.tensor_copy(cpos_tok[:].rearrange("p a b -> p (a b)"), b.t[:, 0:128]))
                ps_free(b, ev)
                for e_ in engs:
                    e_.wait((s_m0, 48))
                barrier()

            with ExitStack() as st2:
                KT = sb("KT", [128, 4, T], BF16, st2)
                Vt = sb("Vt", [128, 16, 512], BF16, st2)
                NE = 8
                E = [sb(f"fE{i}", [128, 512], BF16, st2) for i in range(NE)]
                EB = [Buf() for _ in range(NE)]
                tmpA = sb("ftmpA", [128, 512], F32, st2)
                tmpR = sb("ftmpR", [128, 512], F32, st2)
                tA, tR = Buf(), Buf()
                linear_fm(P["l0_q"], 8, lambda kk, r: hT[:, kk, rng(r)], lambda m, r, b: copy_evac(QT[:, m, rng(r)], b, scale=0.125))
                linear_fm(P["l0_k"], 8, lambda kk, r: hT[:, kk, rng(r)], lambda m, r, b: copy_evac(KT[:, m, rng(r)], b))
                linear_tm(P["l0_v"], 8, lambda kk, t: hT[:, kk, t * 128:(t + 1) * 128], 16,
                          lambda ci, t, b, n: copy_evac(Vt[:, t, :], b))
                barrier()
                state = {"ei": 0}
                COLT = True

                def s_emit(hp, i, r):
                    qlo = max(i * 128, r * 512)
                    n = (r + 1) * 512 - qlo
                    bs_ = [ps_alloc(), ps_alloc()]
                    for b in bs_:
                        b.buf.wr(PE)
                    for a in range(2):
                        nc.tensor.matmul(bs_[a].t[:, 0:n], lhsT=KT[a * 64:a * 64 + 64, hp, i * 128:(i + 1) * 128],
                                         rhs=QT[a * 64:a * 64 + 64, hp, qlo:qlo + n], start=True, stop=False)
                    for a in range(2):
                        ins = nc.tensor.matmul(bs_[a].t[:, 0:n], lhsT=sel72[:, 2 * hp + a, :], rhs=cq3[:, qlo:qlo + n], start=False, stop=True)
                    ev = PE.mark(ins)
                    eis = []
                    for a in range(2):
                        b = bs_[a]
                        b.buf.wr_done(ev)
                        e_i = state["ei"] % NE
                        state["ei"] += 1
                        b.buf.rd(ACT)
                        EB[e_i].wr(ACT)
                        ev2 = ACT.mark(nc.scalar.activation(out=E[e_i][:, 0:n], in_=b.t[:, 0:n], func=AF.Exp,
                                                            bias=cpos_tok[:, i, 2 * hp + a:2 * hp + a + 1], scale=1.0))
                        EB[e_i].wr_done(ev2)
                        ps_free(b, ev2)
                        if i * 128 >= r * 512:
                            DVE.wait(ev2)
                            ev3 = DVE.mark(nc.vector.tensor_tensor(out=E[e_i][:, 0:128], in0=E[e_i][:, 0:128], in1=tri_bf[:], op=ALU.mult))
                            EB[e_i].wr_done(ev3)
                        eis.append(e_i)
                    return (hp, i, r, eis, qlo, n)

                acc = {}

                def pv_emit(item):
                    hp, i, r, eis, qlo, n = item
                    last = 4 * r + 3
                    if i == 0:
                        acc["num"] = ps_alloc()
                        acc["den"] = ps_alloc()
                        acc["num"].buf.wr(PE)
                        acc["den"].buf.wr(PE)
                    num, den = acc["num"], acc["den"]
                    o0 = qlo - r * 512
                    for a in range(2):
                        EB[eis[a]].rd(PE)
                    for a in range(2):
                        rs = slice(a * 64, a * 64 + 64)
                        nc.tensor.matmul(num.t[rs, o0:o0 + n], lhsT=Vt[:, i, hp * 128 + a * 64:hp * 128 + a * 64 + 64], rhs=E[eis[a]][:, 0:n],
                                         start=(i == 0), stop=(i == last), tile_position=(0, a * 64))
                    for a in range(2):
                        rs = slice(a * 64, a * 64 + 64)
                        ins = nc.tensor.matmul(den.t[rs, o0:o0 + n], lhsT=ones_bf[:, 0:64], rhs=E[eis[a]][:, 0:n],
                                               start=(i == 0), stop=(i == last), tile_position=(0, a * 64))
                    ev = PE.mark(ins)
                    for a in range(2):
                        EB[eis[a]].rd_done(ev)
                    if i == last:
                        num.buf.wr_done(ev)
                        den.buf.wr_done(ev)
                        den.buf.rd(ACT)
                        ev_ln, ev_r = recip_act(tmpR[:], den.t[:, :], tmpA[:], tA, tR)
                        ps_free(den, ev_ln)
                        num.buf.rd(DVE)
                        tR.rd(DVE)
                        ev = DVE.mark(nc.vector.tensor_tensor(out=QT[:, hp, rng(r)], in0=num.t[:, :], in1=tmpR[:], op=ALU.mult))
                        tR.rd_done(ev)
                        ps_free(num, ev)

                for r in range(NR):
                    queue = []
                    for hp in range(4):
                        for i in range(4 * r + 4):
                            queue.append(s_emit(hp, i, r))
                            if len(queue) > 3:
                                pv_emit(queue.pop(0))
                    while queue:
                        pv_emit(queue.pop(0))
                barrier()

            with ExitStack() as st2:
                uT = sb("uT", [128, 4, T], BF16, st2)
                vtok = sb("vtok", [128, 16, 512], BF16, st2)
                gjunk = sb("gjunk", [128, 128], F32, st2)
                wsT_f = sb("wsT_f", [128, 512], F32, st2)
                wsT = sb("wsT_b", [128, 4, 128], BF16, st2)
                bs_f = sb("bs_f", [1, 512], F32, st2)
                bs_hi = sb("bs_hi", [1, 512], BF16, st2)
                bs_lo = sb("bs_lo", [1, 512], BF16, st2)
                bs_r = sb("bs_r", [1, 512], F32, st2)
                sgu = sb("sgu_s", [128, 512], F32, st2)
                s_m0b = sem("s_m0b")
                for dst, src in [(wsT_f[:], wsT_d[:, :]), (bs_f[:], bs_d[:, :]), (sgu[:], sgu_d[:, :])]:
                    nc.sync.dma_start(out=dst, in_=src).then_inc(s_m0b, 16)
                for e_ in (ACT, DVE, PE):
                    e_.wait((s_m0b, 48))
                for g in range(4):
                    DVE.mark(nc.vector.tensor_tensor(out=wsT[:, g, :], in0=wsT_f[:, g * 128:(g + 1) * 128], in1=tri[:], op=ALU.mult))
                DVE.mark(nc.vector.tensor_copy(bs_hi[:], bs_f[:]))
                DVE.wait(DVE.now())
                DVE.mark(nc.vector.tensor_tensor(out=bs_r[:], in0=bs_f[:], in1=bs_hi[:], op=ALU.subtract))
                DVE.wait(DVE.now())
                DVE.mark(nc.vector.tensor_copy(bs_lo[:], bs_r[:]))
                barrier()

                gss_all = sb("gss_all", [128, 64], F32, st2)

                def vb_evac(ci, t, b, n):
                    b.buf.rd(ACT)
                    ev = ACT.mark(nc.scalar.activation(out=vtok[:, t, :], in_=b.t[:, 0:512], func=AF.Gelu_apprx_tanh))
                    ACT.wait(ev)
                    for g in range(4):
                        ACT.mark(nc.scalar.activation(out=gjunk[:], in_=vtok[:, t, g * 128:(g + 1) * 128], func=AF.Square,
                                                      accum_out=gss_all[:, t * 4 + g:t * 4 + g + 1]))
                    return [ev]

                linear_tm(P["l0_vb"], 8, lambda kk, t: hT[:, kk, t * 128:(t + 1) * 128], 16, vb_evac)
                ACT.wait(ACT.now())
                ev = ACT.mark(nc.scalar.activation(out=gss_all[:], in_=gss_all[:], func=AF.Ln, bias=EPS, scale=1.0 / 128))
                ACT.wait(ev)
                ev = ACT.mark(nc.scalar.activation(out=gss_all[:], in_=gss_all[:], func=AF.Exp, scale=-0.5))
                DVE.wait(ev)
                for t in range(16):
                    for g in range(4):
                        DVE.mark(nc.vector.scalar_tensor_tensor(out=vtok[:, t, g * 128:(g + 1) * 128], in0=vtok[:, t, g * 128:(g + 1) * 128],
                                                                scalar=gss_all[:, t * 4 + g:t * 4 + g + 1], in1=sgu[:, g * 128:(g + 1) * 128],
                                                                op0=ALU.mult, op1=ALU.mult))
                linear_fm(P["l0_u"], 8, lambda kk, r: hT[:, kk, rng(r)],
                          lambda m, r, b: gelu_evac(uT[:, m, rng(r)], b, 512))
                barrier()
                for g in range(4):
                    for r in range(NR):
                        b = ps_alloc()
                        b.buf.wr(PE)
                        for j in range(4):
                            t = 4 * r + j
                            o = b.t[:, j * 128:(j + 1) * 128]
                            nc.tensor.matmul(o, lhsT=vtok[:, t, g * 128:(g + 1) * 128], rhs=wsT[:, g, :], start=True, stop=False)
                            nc.tensor.matmul(o, lhsT=ones_bf[0:1, :], rhs=bs_hi[0:1, g * 128:(g + 1) * 128], start=False, stop=False)
                            ins = nc.tensor.matmul(o, lhsT=ones_bf[0:1, :], rhs=bs_lo[0:1, g * 128:(g + 1) * 128], start=False, stop=True)
                        ev = PE.mark(ins)
                        b.buf.wr_done(ev)
                        b.buf.rd(DVE)
                        ev = DVE.mark(nc.vector.tensor_tensor(out=uT[:, g, rng(r)], in0=b.t[:, :], in1=uT[:, g, rng(r)], op=ALU.mult))
                        ps_free(b, ev)
                barrier()
                linear_fm(P["l0_wo"], 8, lambda kk, r: (QT[:, kk, rng(r)] if kk < 4 else uT[:, kk - 4, rng(r)]), resid_evac)
                barrier()

    def mixer1():
        with ExitStack() as st:
            hT = sb("hT", [128, KC, T], BF16, st)
            rmsnorm_fm(G_OD, hT, st)
            QT = sb("QT", [128, 4, T], BF16, st)
            convw = sb("convw_s", [128, 4, 3], F32, st)
            neglam = sb("neglam", [128, 1], F32, st)
            subln = sb("subln_s", [128, 1], F32, st)
            ebt = sb("ebt", [128, 8, 128], BF16, st)
            s_m1 = sem("s_m1")
            with ExitStack() as st2:
                lamv = sb("lamv_s", [128, 4, 64], F32, st2)
                lprod = sb("lprod", [128, 2, 64], F32, st2)
                lsum = sb("lsum", [128, 2], F32, st2)
                btab = sb("btab_s", [128, 8, 128], F32, st2)
                rel31 = sb("rel31_s", [128, 4], F32, st2)
                nm1 = 0
                for dst, src in [(convw[:].rearrange("p a b -> p (a b)"), convw_d[:, :]), (lamv[:].rearrange("p a b -> p (a b)"), lamv_d[:, :]),
                                 (subln[:], subln_d[:, :]), (btab[:].rearrange("p a b -> p (a b)"), btab_d[:, :]), (rel31[:], rel31_d[:, :])]:
                    nc.sync.dma_start(out=dst, in_=src).then_inc(s_m1, 16)
                    nm1 += 1
                for e_ in (ACT, DVE):
                    e_.wait((s_m1, 16 * nm1))
                DVE.mark(nc.vector.tensor_tensor(out=lprod[:, 0, :], in0=lamv[:, 0, :], in1=lamv[:, 1, :], op=ALU.mult))
                DVE.mark(nc.vector.tensor_tensor(out=lprod[:, 1, :], in0=lamv[:, 2, :], in1=lamv[:, 3, :], op=ALU.mult))
                DVE.wait(DVE.now())
                DVE.mark(nc.vector.reduce_sum(out=lsum[:], in_=lprod[:], axis=AX.X))
                ACT.wait(DVE.now())
                ev = ACT.mark(nc.scalar.activation(out=lsum[:], in_=lsum[:], func=AF.Exp))
                DVE.wait(ev)
                DVE.mark(nc.vector.tensor_tensor(out=neglam[:], in0=lsum[:, 1:2], in1=lsum[:, 0:1], op=ALU.subtract))
                DVE.wait(DVE.now())
                DVE.mark(nc.vector.tensor_scalar(out=neglam[:], in0=neglam[:], scalar1=-LAM_INIT, scalar2=None, op0=ALU.add))
                DVE.mark(nc.vector.tensor_scalar(out=subln[:], in0=subln[:], scalar1=(1.0 - LAM_INIT), scalar2=None, op0=ALU.mult))
                for h in range(4):
                    for w in range(2):
                        DVE.mark(nc.vector.tensor_scalar(out=btab[:, h * 2 + w, :], in0=btab[:, h * 2 + w, :], scalar1=rel31[:, h:h + 1], scalar2=None,
                                                         op0=ALU.subtract))
                ACT.wait(DVE.now())
                ev = ACT.mark(nc.scalar.activation(out=btab[:].rearrange("p a b -> p (a b)"), in_=btab[:].rearrange("p a b -> p (a b)"), func=AF.Exp))
                DVE.wait(ev)
                for h in range(4):
                    DVE.mark(nc.vector.tensor_tensor(out=ebt[:, h * 2, :], in0=btab[:, h * 2, :], in1=tri[:], op=ALU.mult))
                    DVE.mark(nc.vector.tensor_copy(ebt[:, h * 2 + 1, :], btab[:, h * 2 + 1, :]))
                barrier()

            with ExitStack() as st2:
                KT = sb("KT", [128, 4, T], BF16, st2)
                Vt = sb("Vt", [128, 16, 512], BF16, st2)
                NE = 8
                E = [sb(f"dE{i}", [128, 512], BF16, st2) for i in range(NE)]
                EB = [Buf() for _ in range(NE)]
                tmpA = sb("dtmpA", [128, 512], F32, st2)
                rr = [sb(f"drr{i}", [128, 512], F32, st2) for i in range(2)]
                tA, rB = Buf(), [Buf(), Buf()]
                o_t = sb("do_t", [128, 512], F32, st2)
                o_sq = sb("do_sq", [128, 512], BF16, st2)
                oB, sqB = Buf(), Buf()
                linear_fm(P["l1_q"], 8, lambda kk, r: hT[:, kk, rng(r)], lambda m, r, b: copy_evac(QT[:, m, rng(r)], b, scale=0.125))
                linear_fm(P["l1_k"], 8, lambda kk, r: hT[:, kk, rng(r)], lambda m, r, b: copy_evac(KT[:, m, rng(r)], b))
                linear_tm(P["l1_v"], 8, lambda kk, t: hT[:, kk, t * 128:(t + 1) * 128], 16,
                          lambda ci, t, b, n: copy_evac(Vt[:, t, :], b))
                barrier()
                state = {"ei": 0}

                def s_emit(h, i, r):
                    qlo = max(i * 128, r * 512)
                    n = (r + 1) * 512 - qlo
                    eis = []
                    for br in range(2):
                        ps_ = slice(br * 64, br * 64 + 64)
                        b = ps_alloc()
                        b.buf.wr(PE)
                        ev = PE.mark(nc.tensor.matmul(b.t[:, 0:n], lhsT=KT[ps_, h, i * 128:(i + 1) * 128], rhs=QT[ps_, h, qlo:qlo + n],
                                                      start=True, stop=True))
                        b.buf.wr_done(ev)
                        e_i = state["ei"] % NE
                        state["ei"] += 1
                        b.buf.rd(ACT)
                        EB[e_i].wr(ACT)
                        ev = ACT.mark(nc.scalar.activation(out=E[e_i][:, 0:n], in_=b.t[:, 0:n], func=AF.Exp))
                        EB[e_i].wr_done(ev)
                        ps_free(b, ev)
                        fixes = []
                        if i * 128 >= r * 512:
                            fixes.append((0, 0))
                            if n > 128:
                                fixes.append((128, 1))
                        elif i == 4 * r - 1:
                            fixes.append((0, 1))
                        for (c0, w) in fixes:
                            DVE.wait(EB[e_i].w)
                            ev = DVE.mark(nc.vector.tensor_tensor(out=E[e_i][:, c0:c0 + 128], in0=E[e_i][:, c0:c0 + 128], in1=ebt[:, h * 2 + w, :], op=ALU.mult))
                            EB[e_i].wr_done(ev)
                        eis.append(e_i)
                    return (h, i, r, eis, qlo, n)

                acc = {}

                def pv_emit(item):
                    h, i, r, eis, qlo, n = item
                    last = 4 * r + 3
                    if i == 0:
                        for nm in ("n1", "d1", "n2", "d2"):
                            acc[nm] = ps_alloc()
                            acc[nm].buf.wr(PE)
                    o0 = qlo - r * 512
                    for br in range(2):
                        e_i = eis[br]
                        num, den = acc[f"n{br + 1}"], acc[f"d{br + 1}"]
                        EB[e_i].rd(PE)
                        nc.tensor.matmul(num.t[:, o0:o0 + n], lhsT=Vt[:, i, h * 128:(h + 1) * 128], rhs=E[e_i][:, 0:n], start=(i == 0), stop=(i == last))
                        ev = PE.mark(nc.tensor.matmul(den.t[:, o0:o0 + n], lhsT=ones_bf[:, :], rhs=E[e_i][:, 0:n], start=(i == 0), stop=(i == last)))
                        EB[e_i].rd_done(ev)
                    if i == last:
                        for nm in ("n1", "d1", "n2", "d2"):
                            acc[nm].buf.wr_done(ev)
                        for br in range(2):
                            den = acc[f"d{br + 1}"]
                            den.buf.rd(ACT)
                            ev_ln, ev_r = recip_act(rr[br][:], den.t[:, :], tmpA[:], tA, rB[br])
                            ps_free(den, ev_ln)
                        for br in range(2):
                            num = acc[f"n{br + 1}"]
                            num.buf.rd(DVE)
                            rB[br].rd(DVE)
                            ev = DVE.mark(nc.vector.tensor_tensor(out=rr[br][:], in0=num.t[:, :], in1=rr[br][:], op=ALU.mult))
                            rB[br].wr_done(ev)
                            ps_free(num, ev)
                        rB[0].rd(DVE)
                        rB[1].rd(DVE)
                        oB.wr(DVE)
                        ev = DVE.mark(nc.vector.scalar_tensor_tensor(out=o_t[:], in0=rr[1][:], scalar=neglam[:, 0:1], in1=rr[0][:], op0=ALU.mult, op1=ALU.add))
                        rB[0].rd_done(ev)
                        rB[1].rd_done(ev)
                        oB.wr_done(ev)
                        dq.append([DLY_A, (lambda h=h, r=r: stage2a(h, r))])

                dq = []
                st2bank = {}

                def stage2a(h, r):
                    oB.rd(ACT)
                    sqB.wr(ACT)
                    ev = ACT.mark(nc.scalar.activation(out=o_sq[:], in_=o_t[:], func=AF.Square))
                    oB.rd_done(ev)
                    sqB.wr_done(ev)
                    b = ps_alloc()
                    b.buf.wr(PE)
                    sqB.rd(PE)
                    ev = PE.mark(nc.tensor.matmul(b.t[:, :], lhsT=ones_bf[:, :], rhs=o_sq[:], start=True, stop=True))
                    sqB.rd_done(ev)
                    b.buf.wr_done(ev)
                    st2bank[(h, r)] = b
                    dq.append([DLY_B, (lambda h=h, r=r: stage2b(h, r))])

                def stage2b(h, r):
                    b = st2bank.pop((h, r))
                    b.buf.rd(ACT)
                    tA.wr(ACT)
                    ev = ACT.mark(nc.scalar.activation(out=tmpA[:], in_=b.t[:, :], func=AF.Ln, bias=EPS, scale=1.0 / 128))
                    tA.wr_done(ev)
                    ps_free(b, ev)
                    ACT.wait(ev)
                    rB[0].wr(ACT)
                    ev = ACT.mark(nc.scalar.activation(out=rr[0][:], in_=tmpA[:], func=AF.Exp, scale=-0.5))
                    tA.rd_done(ev)
                    rB[0].wr_done(ev)
                    rB[0].rd(DVE)
                    oB.rd(DVE)
                    ev = DVE.mark(nc.vector.scalar_tensor_tensor(out=QT[:, h, rng(r)], in0=o_t[:], scalar=subln[:, 0:1], in1=rr[0][:], op0=ALU.mult, op1=ALU.mult))
                    rB[0].rd_done(ev)
                    oB.rd_done(ev)

                def tick(flush=False):
                    while True:
                        progressed = False
                        for ent in list(dq):
                            ent[0] -= 1
                        ready = [ent for ent in dq if ent[0] <= 0]
                        for ent in ready:
                            dq.remove(ent)
                            ent[1]()
                            progressed = True
                        if not flush or not dq:
                            break

                for r in range(NR):
                    queue = []
                    for h in range(4):
                        for i in range(4 * r + 4):
                            queue.append(s_emit(h, i, r))
                            tick()
                            if len(queue) > 2:
                                pv_emit(queue.pop(0))
                    while queue:
                        tick()
                        pv_emit(queue.pop(0))
                    tick(flush=True)
                barrier()

            with ExitStack() as st2:
                bgT = sb("bgT", [128, 4, T], BF16, st2)
                cgT = sb("cgT", [128, 4, T], BF16, st2)
                xiT = sb("xiT", [128, 4, T], BF16, st2)
                linear_fm(P["l1_bg"], 8, lambda kk, r: hT[:, kk, rng(r)], lambda m, r, b: copy_evac(bgT[:, m, rng(r)], b))
                linear_fm(P["l1_cg"], 8, lambda kk, r: hT[:, kk, rng(r)], lambda m, r, b: copy_evac(cgT[:, m, rng(r)], b))
                linear_fm(P["l1_xi"], 8, lambda kk, r: hT[:, kk, rng(r)], lambda m, r, b: copy_evac(xiT[:, m, rng(r)], b))
                barrier()
                ev1 = []
                for c in range(4):
                    ev1.append(DVE.mark(nc.vector.tensor_tensor(out=cgT[:, c, :], in0=cgT[:, c, :], in1=xiT[:, c, :], op=ALU.mult)))
                ev2 = []
                for c in range(4):
                    ACT.wait(ev1[c])
                    ev2.append(ACT.mark(nc.scalar.mul(out=xiT[:, c, :], in_=cgT[:, c, :], mul=convw[:, c, 2:3])))
                for c in range(4):
                    DVE.wait(ev2[c])
                    DVE.mark(nc.vector.scalar_tensor_tensor(out=xiT[:, c, 1:T], in0=cgT[:, c, 0:T - 1], scalar=convw[:, c, 1:2], in1=xiT[:, c, 1:T],
                                                            op0=ALU.mult, op1=ALU.add))
                    DVE.wait(DVE.now())
                    DVE.mark(nc.vector.scalar_tensor_tensor(out=xiT[:, c, 2:T], in0=cgT[:, c, 0:T - 2], scalar=convw[:, c, 0:1], in1=xiT[:, c, 2:T],
                                                            op0=ALU.mult, op1=ALU.add))
                    DVE.wait(DVE.now())
                    DVE.mark(nc.vector.tensor_tensor(out=bgT[:, c, :], in0=bgT[:, c, :], in1=xiT[:, c, :], op=ALU.mult))
                barrier()
                linear_fm(P["l1_wo"], 8, lambda kk, r: (bgT[:, kk, rng(r)] if kk < 4 else QT[:, kk - 4, rng(r)]), resid_evac)
                barrier()

    def moe():
        with ExitStack() as st:
            hT = sb("hT", [128, KC, T], BF16, st)
            wr = sb("wr_s", [128, 8, 8], F32, st)
            gwr = sb("gwr", [128, 8, 8], F32, st)
            selE = sb("selE", [8, 8, 128], F32, st)
            z = sb("z", [128, 16, 8], F32, st)
            m8 = sb("m8", [128, 16, 8], F32, st)
            negv1 = sb("negv1", [128, 16], F32, st)
            msk = sb("msk", [128, 16, 8], F32, st)
            ez = sb("ez", [128, 16, 8], F32, st)
            gden = sb("gden", [128, 16], F32, st)
            gT = sb("gT", [8, T], F32, st)
            rtmp = sb("rtmp", [128, 128], F32, st)
            s_mo = sem("s_mo")
            nc.sync.dma_start(out=wr[:].rearrange("p a b -> p (a b)"), in_=wr_d[:, :]).then_inc(s_mo, 16)
            nc.sync.dma_start(out=selE[:].rearrange("p a b -> p (a b)"), in_=sel_d[:, :]).then_inc(s_mo, 16)
            DVE.wait((s_mo, 32))
            PE.wait((s_mo, 32))
            for c in range(8):
                DVE.mark(nc.vector.tensor_scalar(out=gwr[:, c, :], in0=wr[:, c, :], scalar1=gv[:, G_F1, c:c + 1], scalar2=None, op0=ALU.mult))

            def hook(r, rs_tile, rs_buf):
                b = ps_alloc()
                b.buf.wr(PE)
                rs_buf.rd(PE)
                for j in range(4):
                    ins = nc.tensor.transpose(b.t[:, j * 128:(j + 1) * 128], rs_tile[:, j * 128:(j + 1) * 128], ident[:, :])
                ev = PE.mark(ins)
                rs_buf.rd_done(ev)
                b.buf.wr_done(ev)
                b.buf.rd(DVE)
                src = b.t[:, :].rearrange("p (j n) -> p j n", j=4)[:, :, 0:1]
                ev_copy = DVE.mark(nc.vector.tensor_copy(rstd_tok[:, 4 * r:4 * r + 4].rearrange("p (j o) -> p j o", o=1), src))
                ps_free(b, ev_copy)
                PE.wait(ev_gwr)
                b = ps_alloc()
                b.buf.wr(PE)
                for c in range(8):
                    ins = nc.tensor.matmul(b.t[0:8, :], lhsT=gwr[:, c, :], rhs=xT[:, c, rng(r)], start=(c == 0), stop=(c == 7))
                ev = PE.mark(ins)
                b.buf.wr_done(ev)
                b.buf.rd(ACT)
                ev_z = ACT.mark(nc.scalar.copy(out=gT[:, rng(r)], in_=b.t[0:8, :]))
                ps_free(b, ev_z)
                b2 = ps_alloc()
                b2.buf.wr(PE)
                PE.wait(ev_z)
                for j in range(4):
                    t = 4 * r + j
                    ins = nc.tensor.transpose(b2.t[:, j * 8:(j + 1) * 8], gT[:, t * 128:(t + 1) * 128], ident[0:8, 0:8])
                ev = PE.mark(ins)
                b2.buf.wr_done(ev)
                b2.buf.rd(DVE)
                DVE.wait(ev_copy)
                for j in range(4):
                    t = 4 * r + j
                    ev = DVE.mark(nc.vector.tensor_scalar(out=z[:, t, :], in0=b2.t[:, j * 8:(j + 1) * 8], scalar1=rstd_tok[:, t:t + 1], scalar2=None, op0=ALU.mult))
                ps_free(b2, ev)

            ev_gwr = DVE.now()
            rmsnorm_fm(G_F1, hT, st, hook=hook)
            DVE.wait(DVE.now())
            for t in range(16):
                DVE.mark(nc.vector.max(out=m8[:, t, :], in_=z[:, t, :]))
            DVE.wait(DVE.now())
            DVE.mark(nc.vector.tensor_scalar(out=negv1[:].rearrange("p (t o) -> p t o", o=1), in0=m8[:, :, 0:1], scalar1=-1.0, scalar2=None, op0=ALU.mult))
            DVE.mark(nc.vector.tensor_tensor(out=msk[:], in0=z[:], in1=m8[:, :, 1:2].to_broadcast([128, 16, 8]), op=ALU.is_ge))
            DVE.mark(nc.vector.tensor_tensor(out=ez[:], in0=z[:], in1=m8[:, :, 0:1].to_broadcast([128, 16, 8]), op=ALU.subtract))
            ACT.wait(DVE.now())
            ev = ACT.mark(nc.scalar.activation(out=ez[:].rearrange("p a b -> p (a b)"), in_=ez[:].rearrange("p a b -> p (a b)"), func=AF.Exp))
            DVE.wait(ev)
            DVE.wait(DVE.now())
            DVE.mark(nc.vector.tensor_tensor(out=ez[:].rearrange("p a b -> p (a b)"), in0=ez[:].rearrange("p a b -> p (a b)"),
                                             in1=msk[:].rearrange("p a b -> p (a b)"), op=ALU.mult))
            DVE.wait(DVE.now())
            DVE.mark(nc.vector.reduce_sum(out=gden[:], in_=ez[:], axis=AX.X))
            DVE.wait(DVE.now())
            DVE.mark(nc.vector.reciprocal(gden[:], gden[:]))
            DVE.wait(DVE.now())
            DVE.mark(nc.vector.tensor_tensor(out=ez[:], in0=ez[:], in1=gden[:].rearrange("p (t o) -> p t o", o=1).to_broadcast([128, 16, 8]), op=ALU.mult))
            PE.wait(DVE.now())
            for r in range(NR):
                b = ps_alloc()
                b.buf.wr(PE)
                for j in range(4):
                    ins = nc.tensor.transpose(b.t[0:8, j * 128:(j + 1) * 128], ez[:, 4 * r + j, :], ident[:, :])
                ev = PE.mark(ins)
                b.buf.wr_done(ev)
                b.buf.rd(DVE)
                ev = DVE.mark(nc.vector.tensor_copy(gT[:, rng(r)], b.t[0:8, :]))
                ps_free(b, ev)
            barrier()
            hid = sb("hid", [128, 7, T], BF16, st)
            sg = [sb(f"sg{i}", [128, 512], F32, st) for i in range(2)]
            tg = [sb(f"tg{i}", [128, 512], F32, st) for i in range(2)]
            gate_b = [sb(f"gate_b{i}", [128, T], F32, st) for i in range(2)]
            gbB = [Buf(), Buf()]
            bufs = (sg, [Buf(), Buf()], tg, [Buf(), Buf()])
            for e in range(NEXP):
                gb = gate_b[e % 2]
                for r in range(NR):
                    b = ps_alloc()
                    b.buf.wr(PE)
                    ev = PE.mark(nc.tensor.matmul(b.t[:, :], lhsT=selE[:, e, :], rhs=gT[:, rng(r)], start=True, stop=True))
                    b.buf.wr_done(ev)
                    b.buf.rd(ACT)
                    if r == 0:
                        gbB[e % 2].wr(ACT)
                    ev = ACT.mark(nc.scalar.copy(out=gb[:, rng(r)], in_=b.t[:, :]))
                    ps_free(b, ev)
                gbB[e % 2].wr_done(ev)
                gbB[e % 2].rd(DVE)
                ffn_parts(P[f"moe{e}"], hT, hid, bufs, gate_b=gb)
                gbB[e % 2].rd_done(DVE.now())
            barrier()

    def final_out():
        with ExitStack() as st:
            yT = [sb(f"yT{i}", [128, KC, 512], F32, st) for i in range(2)]
            sq = [sb(f"fsq{i}", [128, KC, 512], BF16, st) for i in range(2)]
            lnb = [sb(f"fln{i}", [128, 512], F32, st) for i in range(2)]
            rsb = [sb(f"frs{i}", [128, 512], F32, st) for i in range(2)]
            yB, sqB, lnB, rsB = [Buf(), Buf()], [Buf(), Buf()], [Buf(), Buf()], [Buf(), Buf()]
            NO = 4
            ost = [sb(f"ost{i}", [128, D], F32, st) for i in range(NO)]
            osem = [sem(f"s_ost{i}") for i in range(NO)]
            ocnt = [0] * NO
            oB = [Buf() for _ in range(NO)]
            oi = 0

            def emit_sq(r):
                p = r % 2
                sqB[p].wr(ACT)
                ev = ACT.mark(nc.scalar.activation(out=sq[p][:, :, :], in_=xT[:, :, rng(r)], func=AF.Square))
                sqB[p].wr_done(ev)

            emit_sq(0)
            for r in range(NR):
                p = r % 2
                b = ps_alloc()
                b.buf.wr(PE)
                sqB[p].rd(PE)
                for c in range(KC):
                    ins = nc.tensor.matmul(b.t[:, :], lhsT=ones_bf[:, :], rhs=sq[p][:, c, :], start=(c == 0), stop=(c == KC - 1))
                ev = PE.mark(ins)
                sqB[p].rd_done(ev)
                b.buf.wr_done(ev)
                if r + 1 < NR:
                    emit_sq(r + 1)
                b.buf.rd(ACT)
                lnB[p].wr(ACT)
                ev = ACT.mark(nc.scalar.activation(out=lnb[p][:], in_=b.t[:, :], func=AF.Ln, bias=EPS, scale=1.0 / D))
                lnB[p].wr_done(ev)
                ps_free(b, ev)
                lnB[p].rd(ACT)
                rsB[p].wr(ACT)
                ev = ACT.mark(nc.scalar.activation(out=rsb[p][:], in_=lnb[p][:], func=AF.Exp, scale=-0.5))
                lnB[p].rd_done(ev)
                rsB[p].wr_done(ev)
                rsB[p].rd(DVE)
                yB[p].wr(DVE)
                for c in range(KC):
                    ev = DVE.mark(nc.vector.scalar_tensor_tensor(out=yT[p][:, c, :], in0=xT[:, c, rng(r)], scalar=gv[:, G_FIN, c:c + 1], in1=rsb[p][:],
                                                                 op0=ALU.mult, op1=ALU.mult))
                rsB[p].rd_done(ev)
                yB[p].wr_done(ev)
                for j in range(4):
                    s = oi % NO
                    oi += 1
                    oB[s].wr(ACT)
                    oB[s].wr(DVE)
                    evs = []
                    for half in range(2):
                        b = ps_alloc()
                        b.buf.wr(PE)
                        yB[p].rd(PE)
                        for cc in range(4):
                            c = half * 4 + cc
                            ins = nc.tensor.transpose(b.t[:, cc * 128:(cc + 1) * 128], yT[p][:, c, j * 128:(j + 1) * 128], ident[:, :])
                        ev = PE.mark(ins)
                        yB[p].rd_done(ev)
                        b.buf.wr_done(ev)
                        evs += copy_evac(ost[s][:, half * 512:(half + 1) * 512], b)
                        ps_free(b, evs[-1])
                    SP.wait(*evs)
                    t = 4 * r + j
                    nc.sync.dma_start(out=out_d[t * 128:(t + 1) * 128, :], in_=ost[s][:]).then_inc(osem[s], 16)
                    ocnt[s] += 1
                    oB[s].wr_done(None)
                    oB[s].reads = [(osem[s], 16 * ocnt[s])]
            SP.wait(*[(osem[s], 16 * ocnt[s]) for s in range(NO) if ocnt[s]])

    phases = [("mixer0", mixer0), ("xattn0", lambda: xattn(0)), ("ffn0", ffn_dense),
              ("mixer1", mixer1), ("xattn1", lambda: xattn(1)), ("moe", moe)]
    for name, fn in phases:
        fn()
        if stop_after == name:
            return dump_dbg()
    final_out()
    return nc


def _t5_bucket(dist):
    n = np.maximum(dist, 0)
    nf = np.maximum(n, 1).astype(np.float32)
    large = 16 + (np.log(nf / np.float32(16)) / np.float32(math.log(128 / 16)) * np.float32(16)).astype(np.int32)
    large = np.minimum(large, 31)
    return np.where(n < 16, n, large)


def make_in_maps(inputs):
    f = lambda a: np.ascontiguousarray(np.asarray(a, dtype=np.float32))
    g = {k: np.asarray(v) for k, v in inputs.items()}
    gains = np.stack([g["ev_norm"][0], g["x_norm"][0], g["ffn_norm"][0], g["od_norm"][0], g["x_norm"][1], g["ffn_norm"][1],
                      g["final_norm"]], 0)
    gv = gains.reshape(7, 8, 128).transpose(2, 0, 1).reshape(128, 56)
    kk = np.arange(128)[:, None]
    qq = np.arange(128)[None, :]
    tri = (qq >= kk).astype(np.float32)
    bt = np.zeros((128, 4, 2, 128), np.float32)
    for w in range(2):
        bidx = _t5_bucket(qq - kk + 128 * w)
        bt[:, :, w, :] = np.transpose(g["rel_bias"][bidx], (0, 2, 1))
    sel = np.zeros((8, 8, 128), np.float32)
    for e in range(8):
        sel[e, e, :] = 1.0
    sel72 = np.zeros((128, 8, 128), np.float32)
    for e in range(8):
        for o in (0, 32, 64):
            sel72[o + e, e, :] = 1.0
    common = {
        "ident": np.eye(128, dtype=np.float32),
        "tri": tri,
        "gv": f(gv),
        "memg": f(np.broadcast_to(g["mem_norm"][None, :], (128, D))),
        "bf8": f(g["ev_b_f"][0].reshape(8, 1)),
        "sgu": f(np.broadcast_to(g["ev_sgu_norm"][0].reshape(1, 512), (128, 512))),
        "wsT": f(g["ev_w_s"][0].transpose(2, 0, 1).reshape(128, 512)),
        "bs": f(g["ev_b_s"][0].reshape(1, 512)),
        "convw": f(g["od_conv_w"][0].reshape(3, 4, 128).transpose(2, 1, 0).reshape(128, 12)),
        "lamv": f(np.broadcast_to(np.concatenate([g["od_lam_q1"][0], g["od_lam_k1"][0], g["od_lam_q2"][0], g["od_lam_k2"][0]])[None, :], (128, 256))),
        "subln": f(g["od_subln"][0].reshape(128, 1)),
        "btab": f(bt.reshape(128, 1024)),
        "rel31": f(np.broadcast_to(g["rel_bias"][31][None, :], (128, 4))),
        "wr": f(g["moe_router"][0].reshape(8, 128, 8).transpose(1, 0, 2).reshape(128, 64)),
        "sel": f(sel.reshape(8, 1024)),
        "sel72": f(sel72.reshape(128, 1024)),
        "ev_w_in": f(g["ev_w_in"][0]), "ev_w_out": f(g["ev_w_out"][0]),
        "ffn_w13": f(g["ffn_w13"][0]), "ffn_w2": f(g["ffn_w2"][0]),
        "od_w_in": f(g["od_w_in"][0]), "od_w_out": f(g["od_w_out"][0]),
        "moe_w13": f(g["moe_w13"][0].reshape(NEXP * D, 2 * FF_EXP)),
        "moe_w2": f(g["moe_w2"][0].reshape(NEXP * FF_EXP, D)),
        "x_wq": f(g["x_wq"].reshape(2 * D, 512)), "x_wkv": f(g["x_wkv"].reshape(2 * D, D)),
        "x_wo": f(g["x_wo"].reshape(2 * 512, D)),
    }
    maps = []
    for c in range(8):
        m = dict(common)
        m["x"] = f(g["x"][c])
        m["mem"] = f(g["mem"][c])
        maps.append(m)
    return maps


def kernel(**inputs):
    nc = build_nc()
    in_maps = make_in_maps(inputs)
    res = run_bass_kernel_spmd(nc, in_maps, core_ids=list(range(8)))
    return np.stack([np.asarray(r["out"], dtype=np.float32) for r in res.results], 0)
```
